# Optimizing a Trainium2 kernel written in Bass

```python
import jax
import jax.numpy as jnp
from jax import lax
import numpy as np

D_MODEL = 1024
BATCH = 16
SEQ = 4096
DEPTH = 4

PLE_DIM = 256
D_FF = 2816
EPS = 1e-6
ROPE_THETA = 500000.0
ROPE_FRACTION = 4
NEG_BIG = -1e30

A_HEAD_DIM = 64
A_HEADS_PER_GROUP = 4
A_GROUPS = ((128, 1), (512, 4), (2048, 16))
A_HEADS = A_HEADS_PER_GROUP * len(A_GROUPS)
A_WIDTH = A_HEADS * A_HEAD_DIM
A_OUT = A_HEADS_PER_GROUP * A_HEAD_DIM

B_HEADS = 8
B_DK = 128
B_DV = 96
B_QK_WIDTH = B_HEADS * B_DK
B_WIDTH = B_HEADS * B_DV
B_CHUNK = 32

C_HEADS = 6
C_NOPE = 128
C_ROPE = 64
C_V = 128
C_Q_RANK = 384
C_KV_RANK = 256
C_ROPE_THETA = 10000.0
C_WIDTH = C_HEADS * C_V
C_QBLOCK = 128

N_BRANCHES = 3
IN_SIZES = (A_WIDTH, A_WIDTH, A_WIDTH, B_QK_WIDTH, B_QK_WIDTH, B_WIDTH, B_WIDTH, C_Q_RANK, C_KV_RANK, C_ROPE, N_BRANCHES * D_MODEL)
IN_WIDTH = sum(IN_SIZES)

kernel_name = 'hybrid_gated_parallel_mixer_trunk'

F32 = jnp.float32


def rmsnorm(x, g):
    xf = x.astype(F32)
    y = xf * lax.rsqrt(jnp.mean(xf * xf, axis=-1, keepdims=True) + EPS)
    return (y * g.astype(F32)).astype(x.dtype)


def swiglu(x, w_up, w_down):
    g, u = jnp.split(x @ w_up, 2, axis=-1)
    return (jax.nn.silu(g) * u) @ w_down


def rope_table(seq, dim, theta):
    inv_freq = 1.0 / (theta ** (jnp.arange(0, dim, 2, dtype=F32) / dim))
    ang = jnp.arange(seq, dtype=F32)[:, None] * inv_freq[None, :]
    return jnp.cos(ang), jnp.sin(ang)


def apply_rope(t, cos, sin):
    t1, t2 = jnp.split(t.astype(F32), 2, axis=-1)
    c = cos[None, :, None, :]
    s = sin[None, :, None, :]
    return jnp.concatenate([t1 * c - t2 * s, t2 * c + t1 * s], axis=-1).astype(t.dtype)


def partial_rope(t, cos, sin):
    rot = 2 * cos.shape[-1]
    return jnp.concatenate([apply_rope(t[..., :rot], cos, sin), t[..., rot:]], axis=-1)


def dilated_group(q, k, v, window, dil, scale):
    Bn, Sn, H, hd = q.shape
    span = window // dil
    n = Sn // dil
    nb = -(-n // span)
    pad = nb * span - n

    def to_blocks(t):
        t = t.reshape(Bn, n, dil, H, hd).transpose(0, 2, 1, 3, 4)
        t = jnp.pad(t, ((0, 0), (0, 0), (0, pad), (0, 0), (0, 0)))
        return t.reshape(Bn, dil, nb, span, H, hd)

    def with_prev(t):
        prev = jnp.pad(t, ((0, 0), (0, 0), (1, 0), (0, 0), (0, 0), (0, 0)))[:, :, :-1]
        return jnp.concatenate([prev, t], axis=3)

    qb = to_blocks(q)
    kw = with_prev(to_blocks(k))
    vw = with_prev(to_blocks(v))
    s = jnp.einsum('brnqhd,brnkhd->brnhqk', qb, kw).astype(F32) * scale
    qi = jnp.arange(span)[:, None]
    kj = jnp.arange(2 * span)[None, :]
    dist = qi + span - kj
    key_sub = (jnp.arange(nb) * span)[:, None, None] + kj[None] - span
    mask = (dist >= 0)[None] & (dist <= span)[None] & (key_sub >= 0)
    mask = mask[None, None, :, None]
    s = jnp.where(mask, s, NEG_BIG)
    m = jnp.max(s, axis=-1, keepdims=True)
    e = jnp.where(mask, jnp.exp(s - m), 0.0)
    den = jnp.sum(e, axis=-1, keepdims=True)
    o = jnp.einsum('brnhqk,brnkhd->brnqhd', (e / den).astype(v.dtype), vw)
    lse = (m + jnp.log(den))[..., 0]
    o = o.reshape(Bn, dil, nb * span, H, hd)[:, :, :n].transpose(0, 2, 1, 3, 4).reshape(Bn, Sn, H, hd)
    lse = lse.transpose(0, 1, 2, 4, 3).reshape(Bn, dil, nb * span, H)[:, :, :n]
    lse = lse.transpose(0, 2, 1, 3).reshape(Bn, Sn, H)
    return o, lse


def dilated_window_attention(q, k, v):
    scale = A_HEAD_DIM ** -0.5
    outs, lses = [], []
    for g, (window, dil) in enumerate(A_GROUPS):
        hs = slice(g * A_HEADS_PER_GROUP, (g + 1) * A_HEADS_PER_GROUP)
        o, lse = dilated_group(q[:, :, hs], k[:, :, hs], v[:, :, hs], window, dil, scale)
        outs.append(o)
        lses.append(lse)
    w = jax.nn.softmax(jnp.stack(lses, axis=0), axis=0)
    o = jnp.einsum('gbsh,gbshd->bshd', w, jnp.stack(outs, axis=0).astype(F32))
    return o.astype(q.dtype)


def hgrn2_chunkwise(q, log_f, k, v):
    Bn, Sn, H, dk = q.shape
    L = B_CHUNK
    N = Sn // L

    def chunk(t):
        return t.reshape(Bn, N, L, H, t.shape[-1]).transpose(1, 0, 3, 2, 4)

    q, log_f, k, v = chunk(q), chunk(log_f), chunk(k), chunk(v)
    cum = jnp.cumsum(log_f, axis=3)
    ref = cum[:, :, :, L // 2 - 1:L // 2]
    a = jnp.einsum('nbhld,nbhmd->nbhlm', q * jnp.exp(cum - ref), k * jnp.exp(ref - cum))
    causal = jnp.tril(jnp.ones((L, L), dtype=bool))
    o_intra = jnp.einsum('nbhlm,nbhmv->nbhlv', jnp.where(causal, a, 0.0), v)
    total = cum[:, :, :, -1]
    q_inter = q * jnp.exp(cum)
    k_end = k * jnp.exp(total[:, :, :, None] - cum)

    def step(state, xs):
        qi, ke, vc, tot = xs
        o = jnp.einsum('bhld,bhdv->bhlv', qi, state)
        state = state * jnp.exp(tot)[..., None] + jnp.einsum('bhld,bhlv->bhdv', ke, vc)
        return state, o

    s0 = jnp.zeros((Bn, H, dk, v.shape[-1]), F32)
    _, o_inter = lax.scan(step, s0, (q_inter, k_end, v, total))
    o = o_intra + o_inter
    return o.transpose(1, 0, 3, 2, 4).reshape(Bn, Sn, H, v.shape[-1])


def causal_block_attention(q, k, v, scale):
    Bn, Sn, H, dq = q.shape
    dv = v.shape[-1]
    nq = Sn // C_QBLOCK
    qb = q.reshape(Bn, nq, C_QBLOCK, H, dq).transpose(1, 0, 2, 3, 4)
    kpos = jnp.arange(Sn)

    def one(args):
        qi, start = args
        s = jnp.einsum('bqhd,bkhd->bhqk', qi, k).astype(F32) * scale
        qpos = start + jnp.arange(C_QBLOCK)
        s = jnp.where(qpos[:, None] >= kpos[None, :], s, NEG_BIG)
        p = jax.nn.softmax(s, axis=-1)
        return jnp.einsum('bhqk,bkhd->bqhd', p.astype(v.dtype), v)

    o = lax.map(one, (qb, jnp.arange(nq) * C_QBLOCK))
    return o.transpose(1, 0, 2, 3, 4).reshape(Bn, Sn, H, dv)


def token_mixers(h, w_in, lb, b_gnorm, c_q_norm, w_c_qb, c_kv_norm, w_c_kvb,
                 w_branch_a, w_branch_b, w_branch_c, w_out, rope_a, rope_c):
    Bn, Sn, _ = h.shape
    dt = h.dtype
    offsets = np.cumsum(IN_SIZES)[:-1].tolist()
    (a_q, a_k, a_v, b_q, b_f, b_i, b_g, c_q, c_kv, c_kr, gate_logits) = jnp.split(h @ w_in, offsets, axis=-1)

    def heads(t, n_heads):
        return t.reshape(Bn, Sn, n_heads, -1)

    cos_a, sin_a = rope_a
    qa = partial_rope(heads(a_q, A_HEADS), cos_a, sin_a)
    ka = partial_rope(heads(a_k, A_HEADS), cos_a, sin_a)
    ya = dilated_window_attention(qa, ka, heads(a_v, A_HEADS)).reshape(Bn, Sn, A_OUT) @ w_branch_a

    z = heads(b_f, B_HEADS).astype(F32)
    lbh = lb.astype(F32).reshape(B_HEADS, B_DK)
    log_f = jnp.log(lbh + (1.0 - lbh) * jax.nn.sigmoid(z))
    k_in = (1.0 - lbh) * jax.nn.sigmoid(-z)
    yb = hgrn2_chunkwise(heads(b_q, B_HEADS).astype(F32), log_f, k_in, heads(b_i, B_HEADS).astype(F32))
    yb = rmsnorm(yb, b_gnorm) * jax.nn.silu(heads(b_g, B_HEADS).astype(F32))
    yb = yb.reshape(Bn, Sn, B_WIDTH).astype(dt) @ w_branch_b

    cos_c, sin_c = rope_c
    qc = (rmsnorm(c_q, c_q_norm) @ w_c_qb).reshape(Bn, Sn, C_HEADS, C_NOPE + C_ROPE)
    q_pe = apply_rope(qc[..., C_NOPE:], cos_c, sin_c)
    qc = jnp.concatenate([qc[..., :C_NOPE], q_pe], axis=-1)
    kvc = (rmsnorm(c_kv, c_kv_norm) @ w_c_kvb).reshape(Bn, Sn, C_HEADS, C_NOPE + C_V)
    k_pe = apply_rope(c_kr.reshape(Bn, Sn, 1, C_ROPE), cos_c, sin_c)
    kc = jnp.concatenate([kvc[..., :C_NOPE], jnp.broadcast_to(k_pe, (Bn, Sn, C_HEADS, C_ROPE))], axis=-1)
    yc = causal_block_attention(qc, kc, kvc[..., C_NOPE:], (C_NOPE + C_ROPE) ** -0.5)
    yc = yc.reshape(Bn, Sn, C_WIDTH) @ w_branch_c

    gates = jax.nn.sigmoid(gate_logits.astype(F32)).reshape(Bn, Sn, N_BRANCHES, D_MODEL).astype(dt)
    merged = gates[:, :, 0] * ya + gates[:, :, 1] * yb + gates[:, :, 2] * yc
    return merged @ w_out


def setup_inputs(seed: int = 0) -> dict:
    key = jax.random.key(seed)
    ks = jax.random.split(key, 32)
    L = DEPTH
    D = D_MODEL

    def w(k, shape, fan_in):
        return jax.random.normal(k, shape, F32) * fan_in ** -0.5

    def gain(k, shape):
        return 1.0 + 0.05 * jax.random.normal(k, shape, F32)

    return {
        'x': jax.random.normal(ks[0], (BATCH, SEQ, D), F32),
        'p': jax.random.normal(ks[1], (DEPTH, BATCH, SEQ, PLE_DIM), F32),
        'norm_ffn1': gain(ks[2], (L, D)),
        'w_ffn1_up': w(ks[3], (L, D, 2 * D_FF), D),
        'w_ffn1_down': w(ks[4], (L, D_FF, D), D_FF),
        'norm_mix': gain(ks[5], (L, D)),
        'w_in': w(ks[6], (L, D, IN_WIDTH), D),
        'b_lb_logits': 0.1 * jax.random.normal(ks[7], (L, B_QK_WIDTH), F32),
        'b_gnorm': gain(ks[8], (L, B_DV)),
        'c_q_norm': gain(ks[9], (L, C_Q_RANK)),
        'w_c_qb': w(ks[10], (L, C_Q_RANK, C_HEADS * (C_NOPE + C_ROPE)), C_Q_RANK),
        'c_kv_norm': gain(ks[11], (L, C_KV_RANK)),
        'w_c_kvb': w(ks[12], (L, C_KV_RANK, C_HEADS * (C_NOPE + C_V)), C_KV_RANK),
        'w_branch_a': w(ks[13], (L, A_OUT, D), A_OUT),
        'w_branch_b': w(ks[14], (L, B_WIDTH, D), B_WIDTH),
        'w_branch_c': w(ks[15], (L, C_WIDTH, D), C_WIDTH),
        'w_out': w(ks[16], (L, D, D), D),
        'norm_ffn2': gain(ks[17], (L, D)),
        'w_ffn2_up': w(ks[18], (L, D, 2 * D_FF), D),
        'w_ffn2_down': w(ks[19], (L, D_FF, D), D_FF),
        'norm_ple': gain(ks[20], (L, D)),
        'w_ple_gate': w(ks[21], (L, D, D), D),
        'w_ple_proj': w(ks[22], (L, PLE_DIM, D), PLE_DIM),
        'norm_final': gain(ks[23], (D,)),
    }


def reference(x, p, norm_ffn1, w_ffn1_up, w_ffn1_down, norm_mix, w_in, b_lb_logits, b_gnorm,
              c_q_norm, w_c_qb, c_kv_norm, w_c_kvb, w_branch_a, w_branch_b, w_branch_c, w_out,
              norm_ffn2, w_ffn2_up, w_ffn2_down, norm_ple, w_ple_gate, w_ple_proj, norm_final):
    Sn = x.shape[1]
    rope_a = rope_table(Sn, A_HEAD_DIM // ROPE_FRACTION, ROPE_THETA)
    rope_c = rope_table(Sn, C_ROPE, C_ROPE_THETA)
    lb_p = jax.nn.softmax(b_lb_logits.astype(F32), axis=0)
    lower_bounds = jnp.cumsum(lb_p, axis=0) - lb_p[0:1]
    for i in range(DEPTH):
        x = x + 0.5 * swiglu(rmsnorm(x, norm_ffn1[i]), w_ffn1_up[i], w_ffn1_down[i])
        h = rmsnorm(x, norm_mix[i])
        x = x + token_mixers(h, w_in[i], lower_bounds[i], b_gnorm[i], c_q_norm[i], w_c_qb[i],
                             c_kv_norm[i], w_c_kvb[i], w_branch_a[i], w_branch_b[i], w_branch_c[i],
                             w_out[i], rope_a, rope_c)
        x = x + 0.5 * swiglu(rmsnorm(x, norm_ffn2[i]), w_ffn2_up[i], w_ffn2_down[i])
        gate = jax.nn.sigmoid((rmsnorm(x, norm_ple[i]) @ w_ple_gate[i]).astype(F32)).astype(x.dtype)
        x = x + gate * (p[i] @ w_ple_proj[i])
    return rmsnorm(x, norm_final)
```

```python
import contextlib
import os
import numpy as np
import concourse.bass as bass
import concourse.mybir as mybir
from concourse.bass_utils import run_bass_kernel_spmd

F32 = mybir.dt.float32
BF16 = mybir.dt.bfloat16
ALU = mybir.AluOpType
AF = mybir.ActivationFunctionType
AX = mybir.AxisListType

D = 1024
SEQ = 4096
DEPTH = 4
DFF = 2816
EPS = 1e-6
NCORES = 8
TS = 512
O_AQ, O_AK, O_AV, O_BQ, O_BF, O_BI, O_BG, O_CQ, O_CKV, O_CKR, O_G = 0, 768, 1536, 2304, 3328, 4352, 5120, 5888, 6272, 6528, 6592
A_GROUPS = ((128, 1), (512, 4), (2048, 16))


class Sched:
    EPOCH = 30000
    NDS = 16

    def __init__(self, nc, es):
        self.nc = nc
        self.es = es
        self.E = {'pe': nc.tensor, 'act': nc.scalar, 'dve': nc.vector, 'pool': nc.gpsimd, 'sp': nc.sync}
        self.cnt = {e: 0 for e in ('pe', 'act', 'dve', 'pool')}
        self.csem = {e: [] for e in self.cnt}
        self.dsem = [es.enter_context(nc.semaphore("d%d" % i)) for i in range(self.NDS)]
        self.dval = [0] * self.NDS
        self.ndma = 0
        self.wc = {q: {} for q in self.E}
        self.wd = {q: {} for q in self.E}
        self.lw = {}
        self.rd = {}
        self.groups = {}
        self.ninst = 0

    def _csem(self, e, idx):
        ep = (idx - 1) // self.EPOCH
        while len(self.csem[e]) <= ep:
            self.csem[e].append(self.es.enter_context(self.nc.semaphore("c_%s_%d" % (e, len(self.csem[e])))))
        return self.csem[e][ep], idx - ep * self.EPOCH

    def _wait(self, q, tok):
        if tok[0] == 'c':
            _, e, idx = tok
            if e == q and e == 'pe':
                return
            if self.wc[q].get(e, 0) >= idx:
                return
            assert idx <= self.cnt[e], "wait on unsignaled instr %s %s" % (q, tok)
            s, v = self._csem(e, idx)
            self.E[q].wait_ge(s, v)
            self.wc[q][e] = idx
        else:
            _, j, v = tok
            if self.wd[q].get(j, 0) >= v:
                return
            self.E[q].wait_ge(self.dsem[j], v)
            self.wd[q][j] = v
        self.ninst += 1

    def _deps(self, q, reads, writes, is_dma):
        for k in reads:
            for kk in self.groups.get(k, (k,)):
                t = self.lw.get(kk)
                if t is not None:
                    self._wait(q, t)
        for k in writes:
            t = self.lw.get(k)
            if t is not None and (is_dma or not (t[0] == 'c' and t[1] == q)):
                self._wait(q, t)
            for t2 in self.rd.get(k, {}).values():
                if is_dma or not (t2[0] == 'c' and t2[1] == q):
                    self._wait(q, t2)

    def op(self, e, fn, reads=(), writes=(), signal=True):
        psr = [k for k in reads if isinstance(k, tuple) and k[0] == 'ps']
        if psr:
            reads = [k for k in reads if k not in psr]
            writes = list(writes) + psr
        self._deps(e, reads, writes, False)
        ins = fn()
        self.ninst += 1
        if signal:
            self.cnt[e] += 1
            idx = self.cnt[e]
            s, v = self._csem(e, idx)
            ins.then_inc(s, 1)
        else:
            idx = self.cnt[e] + 1
        tok = ('c', e, idx)
        for k in reads:
            self.rd.setdefault(k, {})[e] = tok
        for k in writes:
            self.lw[k] = tok
            self.rd[k] = {}
        return ins

    def dma(self, q, out, in_, reads=(), writes=()):
        i = self.ndma
        self.ndma += 1
        j = i % self.NDS
        if self.dval[j] > 0:
            self._wait(q, ('d', j, self.dval[j]))
        self._deps(q, reads, writes, True)
        self.dval[j] += 16
        self.E[q].dma_start(out=out, in_=in_).then_inc(self.dsem[j], 16)
        self.ninst += 1
        tok = ('d', j, self.dval[j])
        for k in reads:
            self.rd.setdefault(k, {})[('d', j)] = tok
        for k in writes:
            self.lw[k] = tok
            self.rd[k] = {}

    def barrier(self):
        for q in self.E:
            for e in self.cnt:
                if e != q and self.cnt[e] > 0:
                    self._wait(q, ('c', e, self.cnt[e]))
            for j in range(self.NDS):
                if self.dval[j] > 0:
                    self._wait(q, ('d', j, self.dval[j]))
        self.lw = {}
        self.rd = {}
        self.groups = {}


class K:
    def __init__(self, nc, nseq, depth, dump=()):
        self.nc = nc
        self.nseq = nseq
        self.T = nseq * SEQ
        self.NT = self.T // TS
        self.depth = depth
        self.dump = set(dump)
        self.es = contextlib.ExitStack()
        self.S = Sched(nc, self.es)
        self.ps = self.es.enter_context(nc.psum_tensor("ps", [128, 4096], F32))
        self.dram = {}

    def bank(self, b, n=512, p=128):
        return self.ps[0:p, b * 512:b * 512 + n]

    def dt(self, name, shape, dtype, kind=None):
        if kind is None:
            kind = "ExternalOutput" if name in self.dump else "Internal"
        t = self.nc.dram_tensor(name, list(shape), dtype, kind=kind).ap()
        self.dram[name] = t
        return t

    def sb(self, st, name, shape, dtype):
        self.uid = getattr(self, 'uid', 0) + 1
        return st.enter_context(self.nc.sbuf_tensor("s%d_%s" % (self.uid, name), list(shape), dtype))

    def mm_group(self, out, pairs, reads, wkey):
        n = len(pairs)
        for i, (l, r) in enumerate(pairs):
            self.S.op('pe', lambda l=l, r=r, i=i: self.nc.tensor.matmul(out, l, r, start=(i == 0), stop=(i == n - 1)),
                      reads=reads, writes=[wkey], signal=(i == n - 1))

    def load_w(self, dst, src_rows, c0, c1, key, kc_n, dcol=0):
        for kc in range(kc_n):
            self.S.dma('pool', dst[:, kc, dcol:dcol + (c1 - c0)], src_rows[kc * 128:(kc + 1) * 128, c0:c1], writes=[(key, kc, dcol)])
        self.S.groups[key] = list(self.S.groups.get(key, [])) + [(key, kc, dcol) for kc in range(kc_n)]

    def rmsnorm(self, x, KC, Dn, g, out, keys_in, key_out, sq, rstd, kp, pb):
        nc, S = self.nc, self.S
        N = x.shape[-1]
        S.op('act', lambda: nc.scalar.activation(sq, x, AF.Square), reads=keys_in, writes=[kp + 'sq'])
        bk = self.bank(pb, N)
        self.mm_group(bk, [(self.ones[:], sq[:, kc, :]) for kc in range(KC)], [kp + 'sq', 'const'], ('ps', pb))
        S.op('dve', lambda: nc.vector.tensor_scalar(rstd, bk, 1.0 / Dn, EPS, ALU.mult, ALU.add), reads=[('ps', pb)], writes=[kp + 'rs0'])
        S.op('act', lambda: nc.scalar.activation(rstd, rstd, AF.Sqrt), reads=[kp + 'rs0'], writes=[kp + 'rs1'])
        S.op('dve', lambda: nc.vector.reciprocal(rstd, rstd), reads=[kp + 'rs1'], writes=[kp + 'rstd'])
        for kc in range(KC):
            S.op('dve', lambda kc=kc: nc.vector.scalar_tensor_tensor(out[:, kc, :], x[:, kc, :], g[:, kc:kc + 1], rstd, ALU.mult, ALU.mult),
                 reads=keys_in + [kp + 'rstd', 'const'], writes=[key_out])

    def setup_consts(self, cin):
        nc, S = self.nc, self.S
        st = self.es
        self.ones = self.sb(st, "ones", [128, 128], BF16)
        self.ident = self.sb(st, "ident", [128, 128], BF16)
        self.mcur = self.sb(st, "mcur", [128, 128], BF16)
        self.mprev = self.sb(st, "mprev", [128, 128], BF16)
        self.permA = self.sb(st, "permA", [128, 128], BF16)
        self.permC = self.sb(st, "permC", [128, 128], BF16)
        self.scanm = self.sb(st, "scanm", [128, TS], F32)
        S.op('dve', lambda: nc.vector.memset(self.ones[:], 1.0), writes=['const'])
        for t, nm in ((self.ident, 'ident'), (self.mcur, 'mcur'), (self.mprev, 'mprev'), (self.permA, 'permA'), (self.permC, 'permC')):
            S.dma('pool', t[:], cin[nm], writes=['const'])
        S.dma('sp', self.scanm[:], cin['scanm'], writes=['const'])

    def phase_ffn(self, l, src, dst, w_up, w_dn, g_ffn, g_post=None, ht_dst=None):
        nc, S = self.nc, self.S
        with contextlib.ExitStack() as st:
            wup = self.sb(st, "wup", [128, 8, 2 * DFF], BF16)
            wdn = self.sb(st, "wdn", [128, 22, D], BF16)
            gf = self.sb(st, "gf", [128, 8], F32)
            gp = self.sb(st, "gp", [128, 8], F32)
            xt = [self.sb(st, "xt%d" % i, [128, 8, TS], F32) for i in range(2)]
            ht = self.sb(st, "ht", [128, 8, TS], BF16)
            at = self.sb(st, "at", [128, 22, TS], BF16)
            sg = [self.sb(st, "sg%d" % i, [128, TS], F32) for i in range(2)]
            rstd = self.sb(st, "rstd", [128, TS], F32)
            S.dma('sp', gf[:], g_ffn, writes=['const'])
            if g_post is not None:
                S.dma('sp', gp[:], g_post, writes=['const'])
            for half in range(2):
                self.load_w(wup, w_up, half * DFF, (half + 1) * DFF, 'wup', 8, dcol=half * DFF)
            self.load_w(wdn, w_dn, 0, D, 'wdn', 22)
            srcv = src.rearrange("(c p) t -> p c t", p=128)
            dstv = dst.rearrange("(c p) t -> p c t", p=128)
            for tt in range(self.NT):
                x = xt[tt % 2]
                xk = 'xt%d' % (tt % 2)
                tsl = slice(tt * TS, (tt + 1) * TS)
                S.dma('sp', x[:], srcv[:, :, tsl], writes=[xk])
                self.rmsnorm(x[:], 8, D, gf, ht[:], [xk], 'ht', at[:, 0:8, :], rstd[:], 'n1', 6)
                for j in range(22):
                    bg, bu = 2 * (j % 2), 2 * (j % 2) + 1
                    self.mm_group(self.bank(bg), [(wup[:, kc, j * 128:(j + 1) * 128], ht[:, kc, :]) for kc in range(8)], ['wup', 'ht'], ('ps', bg))
                    self.mm_group(self.bank(bu), [(wup[:, kc, DFF + j * 128:DFF + (j + 1) * 128], ht[:, kc, :]) for kc in range(8)], ['wup', 'ht'], ('ps', bu))
                    s_ = sg[j % 2]
                    S.op('act', lambda s_=s_, bg=bg: nc.scalar.activation(s_[:], self.bank(bg), AF.Silu), reads=[('ps', bg)], writes=['sg%d' % (j % 2)])
                    S.op('dve', lambda s_=s_, bu=bu, j=j: nc.vector.tensor_tensor(at[:, j, :], s_[:], self.bank(bu), ALU.mult),
                         reads=['sg%d' % (j % 2), ('ps', bu)], writes=['n1sq' if j < 8 else 'at'])
                for dc in range(8):
                    b = 4 + dc % 2
                    self.mm_group(self.bank(b), [(wdn[:, kc, dc * 128:(dc + 1) * 128], at[:, kc, :]) for kc in range(22)], ['wdn', 'at', 'n1sq'], ('ps', b))
                    S.op('dve', lambda dc=dc, b=b: nc.vector.scalar_tensor_tensor(x[:, dc, :], self.bank(b), 0.5, x[:, dc, :], ALU.mult, ALU.add),
                         reads=[('ps', b), xk], writes=[xk])
                S.dma('pool', dstv[:, :, tsl], x[:], reads=[xk])
                if g_post is not None:
                    self.rmsnorm(x[:], 8, D, gp, ht[:], [xk], 'ht', at[:, 0:8, :], rstd[:], 'n1', 6)
                    S.dma('pool', ht_dst.rearrange("(c p) t -> p c t", p=128)[:, :, tsl], ht[:], reads=['ht'])
        S.barrier()

    def phase_ple(self, l, src, dst, pT, w_pg, w_pp, g_ple, g_fin=None, out_dst=None):
        nc, S = self.nc, self.S
        with contextlib.ExitStack() as st:
            wpg = self.sb(st, "wpg", [128, 8, D], BF16)
            wpp = self.sb(st, "wpp", [128, 2, D], BF16)
            gl = self.sb(st, "gl", [128, 8], F32)
            gfn = self.sb(st, "gfn", [128, 8], F32)
            xt = [self.sb(st, "xt%d" % i, [128, 8, TS], F32) for i in range(2)]
            pt = [self.sb(st, "pt%d" % i, [128, 2, TS], BF16) for i in range(2)]
            ht = self.sb(st, "ht", [128, 8, TS], BF16)
            sq = self.sb(st, "sq", [128, 8, TS], BF16)
            xo = self.sb(st, "xo", [128, 8, TS], F32)
            sg = [self.sb(st, "sg%d" % i, [128, TS], F32) for i in range(2)]
            rstd = self.sb(st, "rstd", [128, TS], F32)
            S.dma('sp', gl[:], g_ple, writes=['const'])
            if g_fin is not None:
                S.dma('sp', gfn[:], g_fin, writes=['const'])
            self.load_w(wpg, w_pg, 0, D, 'wpg', 8)
            self.load_w(wpp, w_pp, 0, D, 'wpp', 2)
            srcv = src.rearrange("(c p) t -> p c t", p=128)
            dstv = dst.rearrange("(c p) t -> p c t", p=128)
            pv = pT.rearrange("(c p) t -> p c t", p=128)
            for tt in range(self.NT):
                x = xt[tt % 2]
                xk = 'xt%d' % (tt % 2)
                p_ = pt[tt % 2]
                pk = 'pt%d' % (tt % 2)
                tsl = slice(tt * TS, (tt + 1) * TS)
                S.dma('sp', x[:], srcv[:, :, tsl], writes=[xk])
                S.dma('pool', p_[:], pv[:, :, tsl], writes=[pk])
                self.rmsnorm(x[:], 8, D, gl, ht[:], [xk], 'ht', sq[:], rstd[:], 'n1', 6)
                for dc in range(8):
                    b0, b1 = 2 * (dc % 2), 2 * (dc % 2) + 1
                    self.mm_group(self.bank(b0), [(wpg[:, kc, dc * 128:(dc + 1) * 128], ht[:, kc, :]) for kc in range(8)], ['wpg', 'ht'], ('ps', b0))
                    self.mm_group(self.bank(b1), [(wpp[:, kc, dc * 128:(dc + 1) * 128], p_[:, kc, :]) for kc in range(2)], ['wpp', pk], ('ps', b1))
                    s_ = sg[dc % 2]
                    sk = 'sg%d' % (dc % 2)
                    S.op('act', lambda s_=s_, b0=b0: nc.scalar.activation(s_[:], self.bank(b0), AF.Sigmoid), reads=[('ps', b0)], writes=[sk])
                    S.op('dve', lambda s_=s_, b1=b1: nc.vector.tensor_tensor(s_[:], s_[:], self.bank(b1), ALU.mult), reads=[sk, ('ps', b1)], writes=[sk])
                    S.op('dve', lambda s_=s_, dc=dc: nc.vector.tensor_tensor(x[:, dc, :], x[:, dc, :], s_[:], ALU.add), reads=[sk, xk], writes=[xk])
                if g_fin is None:
                    S.dma('pool', dstv[:, :, tsl], x[:], reads=[xk])
                else:
                    self.rmsnorm(x[:], 8, D, gfn, xo[:], [xk], 'xo', sq[:], rstd[:], 'n1', 6)
                    S.dma('pool', out_dst.rearrange("(c p) t -> p c t", p=128)[:, :, tsl], xo[:], reads=['xo'])
        S.barrier()

    def alloc_scratch(self):
        T = self.T
        for nm, shp, dt_ in (
            ('AQ', [768, T], BF16), ('AK', [768, T], BF16), ('AV', [T, 768], BF16), ('AO', [3, T, 260], F32), ('YA', [256, T], BF16),
            ('BQ1', [1024, T], BF16), ('BK1', [1024, T], BF16), ('BQ2', [1024, T], BF16), ('BKE', [T, 1024], BF16),
            ('BDEC', [128, 8, T // 32], F32), ('BV', [T, 768], BF16), ('BG', [T, 768], BF16), ('YB', [768, T], BF16),
            ('CQN', [768, T], BF16), ('CQR', [384, T], BF16), ('CKN', [768, T], BF16), ('CKR', [128, T], BF16),
            ('CV', [T, 768], BF16), ('YC', [768, T], BF16)):
            self.dt(nm, shp, dt_)
        nc, S, st = self.nc, self.S, self.es
        self.lbc = self.sb(st, "lbc", [128, 8, DEPTH], F32)
        self.oml = self.sb(st, "oml", [128, 8, DEPTH], F32)
        ex = self.sb(st, "lbex", [128, 8, DEPTH], F32)
        ss = self.sb(st, "lbss", [128, 8], F32)
        self.mask4 = self.sb(st, "mask4", [128, 4, 256], BF16)
        self.mask8 = self.sb(st, "mask8", [32, 8, 32], BF16)
        S.dma('sp', ex[:], self.lbl, writes=['lbex'])
        S.op('act', lambda: nc.scalar.activation(ex[:], ex[:], AF.Exp), reads=['lbex'], writes=['lbex'])
        S.op('dve', lambda: nc.vector.tensor_reduce(ss[:], ex[:], AX.X, ALU.add), reads=['lbex'], writes=['lbss'])
        S.op('dve', lambda: nc.vector.reciprocal(ss[:], ss[:]), reads=['lbss'], writes=['lbss2'])
        S.op('dve', lambda: nc.vector.tensor_tensor(ex[:], ex[:], ss[:].unsqueeze(2).to_broadcast([128, 8, DEPTH]), ALU.mult), reads=['lbss2', 'lbex'], writes=['lbp'])
        S.op('dve', lambda: nc.vector.memset(self.lbc[:, :, 0:1], 0.0), writes=['lbc0'])
        S.op('dve', lambda: nc.vector.tensor_copy(self.lbc[:, :, 1:2], ex[:, :, 1:2]), reads=['lbp'], writes=['lbc1'])
        for i in (2, 3):
            S.op('dve', lambda i=i: nc.vector.tensor_tensor(self.lbc[:, :, i:i + 1], self.lbc[:, :, i - 1:i], ex[:, :, i:i + 1], ALU.add),
                 reads=['lbp', 'lbc%d' % (i - 1)], writes=['lbc%d' % i])
        S.op('dve', lambda: nc.vector.tensor_scalar(self.oml[:], self.lbc[:], -1.0, 1.0, ALU.mult, ALU.add), reads=['lbc3', 'lbc0', 'lbc1', 'lbc2'], writes=['const'])
        for h4 in range(4):
            S.op('pool', lambda h4=h4: nc.gpsimd.tensor_copy(self.mask4[:, h4, 0:128], self.mprev[:]), reads=['const'], writes=['const2'])
            S.op('pool', lambda h4=h4: nc.gpsimd.tensor_copy(self.mask4[:, h4, 128:256], self.mcur[:]), reads=['const'], writes=['const2'])
        for h in range(8):
            S.op('pool', lambda h=h: nc.gpsimd.tensor_copy(self.mask8[:, h, :], self.mcur[0:32, 0:32]), reads=['const'], writes=['const2'])

    def rope(self, bq, bp, tc, ts, perm, out, qb, t1, t2, kq, okey):
        nc, S = self.nc, self.S
        S.op('act', lambda: nc.scalar.copy(qb[:], self.bank(bq)), reads=[('ps', bq)], writes=['qb'])
        self.mm_group(self.bank(bp), [(perm[:], qb[:])], ['qb', 'const'], ('ps', bp))
        S.op('dve', lambda: nc.vector.tensor_tensor(t1[:], self.bank(bq), tc, ALU.mult), reads=[('ps', bq), kq], writes=['t1'])
        S.op('dve', lambda: nc.vector.tensor_tensor(t2[:], self.bank(bp), ts, ALU.mult), reads=[('ps', bp), kq], writes=['t2'])
        S.op('pool', lambda: nc.gpsimd.tensor_tensor(out, t1[:], t2[:], ALU.add), reads=['t1', 't2'], writes=[okey])

    def fm(self, ap2d):
        return ap2d.rearrange("(c p) t -> p c t", p=128)

    def phase_mixer(self, l, XT, HT, W, V, lbl):
        import os
        ph = os.environ.get('MIXPH', 'a,b,c,3,4,5,6,7').split(',')
        if 'a' in ph:
            self.p2a(l, HT, W['w_in'][l])
        if 'b' in ph:
            self.p2b(l, HT, W['w_in'][l])
        if 'c' in ph:
            self.p2c(l, HT, W['w_in'][l], W['w_c_qb'][l], W['w_c_kvb'][l], V['c_q_norm'][l], V['c_kv_norm'][l])
        if '3' in ph:
            self.p3(l)
        if '4' in ph:
            self.p4(l)
        if '5' in ph:
            self.p5(l)
        if '6' in ph:
            self.p6(l, V['b_gnorm'][l])
        if '7' in ph:
            self.p7(l, XT, HT, W['w_in'][l], W['w_branch_a'][l], W['w_branch_b'][l], W['w_branch_c'][l], W['w_out'][l])

    def p2a(self, l, HT, w_in):
        nc, S = self.nc, self.S
        dr = self.dram
        with contextlib.ExitStack() as st:
            wa = self.sb(st, "wa", [128, 8, 2304], BF16)
            ht = [self.sb(st, "ht%d" % i, [128, 8, TS], BF16) for i in range(2)]
            tc_ = [self.sb(st, "tc%d" % i, [128, TS], F32) for i in range(2)]
            ts_ = [self.sb(st, "ts%d" % i, [128, TS], F32) for i in range(2)]
            qb = self.sb(st, "qb", [128, TS], BF16)
            t1 = self.sb(st, "t1", [128, TS], F32)
            t2 = self.sb(st, "t2", [128, TS], F32)
            oq = [self.sb(st, "oq%d" % i, [128, 12, TS], BF16) for i in range(2)]
            ov = [self.sb(st, "ov%d" % i, [128, 4, 768], BF16) for i in range(2)]
            self.load_w(wa, w_in, 0, 2304, 'wa', 8)
            for tt in range(self.NT):
                i2 = tt % 2
                h, hk = ht[i2], 'ht%d' % i2
                tsl = slice(tt * TS, (tt + 1) * TS)
                psl = slice((tt % 8) * TS, (tt % 8 + 1) * TS)
                S.dma('sp', h[:], self.fm(HT)[:, :, tsl], writes=[hk])
                S.dma('sp', tc_[i2][:], self.cin['ropeA_c'][:, psl], writes=['tab%d' % i2])
                S.dma('sp', ts_[i2][:], self.cin['ropeA_s'][:, psl], writes=['tab%d' % i2])
                for c in range(12):
                    bq, bp = 2 * (c % 2), 2 * (c % 2) + 1
                    self.mm_group(self.bank(bq), [(wa[:, kc, c * 128:(c + 1) * 128], h[:, kc, :]) for kc in range(8)], ['wa', hk], ('ps', bq))
                    self.rope(bq, bp, tc_[i2][:], ts_[i2][:], self.permA, oq[i2][:, c, :], qb, t1, t2, 'tab%d' % i2, 'oq%d' % i2)
                S.dma('pool', self.fm(dr['AQ'])[:, :, tsl], oq[i2][:, 0:6, :], reads=['oq%d' % i2])
                S.dma('pool', self.fm(dr['AK'])[:, :, tsl], oq[i2][:, 6:12, :], reads=['oq%d' % i2])
                for sub in range(4):
                    for (c0, n, b) in ((0, 512, 4), (512, 256, 5)):
                        self.mm_group(self.bank(b, n), [(h[:, kc, sub * 128:(sub + 1) * 128], wa[:, kc, 1536 + c0:1536 + c0 + n]) for kc in range(8)], ['wa', hk], ('ps', b))
                        S.op('act', lambda sub=sub, c0=c0, n=n, b=b: nc.scalar.copy(ov[i2][:, sub, c0:c0 + n], self.bank(b, n)), reads=[('ps', b)], writes=['ov%d' % i2])
                S.dma('pool', dr['AV'][tsl, :].rearrange("(j p) f -> p j f", p=128), ov[i2][:], reads=['ov%d' % i2])
        S.barrier()

    def p2b(self, l, HT, w_in):
        nc, S = self.nc, self.S
        dr = self.dram
        with contextlib.ExitStack() as st:
            wb = self.sb(st, "wb", [128, 8, 3584], BF16)
            ht = [self.sb(st, "ht%d" % i, [128, 8, TS], BF16) for i in range(2)]
            f32t = {n: self.sb(st, n, [128, TS], F32) for n in ('ef', 'sgm', 'kin', 'lf', 'cum', 'd1', 'e1', 'e2', 'e3', 'e4')}
            q1s = self.sb(st, "q1s", [128, 8, TS], BF16)
            k1s = self.sb(st, "k1s", [128, 8, TS], BF16)
            q2s = self.sb(st, "q2s", [128, 8, TS], BF16)
            ket = self.sb(st, "ket", [128, TS], BF16)
            kes = self.sb(st, "kes", [128, 4, 1024], BF16)
            decs = self.sb(st, "decs", [128, 8, 16], F32)
            bvs = self.sb(st, "bvs", [128, 4, 768], BF16)
            bgs = self.sb(st, "bgs", [128, 4, 768], BF16)
            ge = self.sb(st, "ge", [128, 512], F32)
            self.load_w(wb, w_in, O_BQ, O_BQ + 3584, 'wb', 8)
            T_ = f32t
            v3 = lambda t: t[:].rearrange("p (c l) -> p c l", l=32)
            for tt in range(self.NT):
                i2 = tt % 2
                h, hk = ht[i2], 'ht%d' % i2
                tsl = slice(tt * TS, (tt + 1) * TS)
                S.dma('sp', h[:], self.fm(HT)[:, :, tsl], writes=[hk])
                for hd in range(8):
                    lb1 = self.lbc[:, hd, l:l + 1]
                    om1 = self.oml[:, hd, l:l + 1]
                    self.mm_group(self.bank(0), [(wb[:, kc, 1024 + hd * 128:1024 + (hd + 1) * 128], h[:, kc, :]) for kc in range(8)], ['wb', hk], ('ps', 0))
                    S.op('act', lambda: nc.scalar.activation(T_['ef'][:], self.bank(0), AF.Exp, scale=-1.0), reads=[('ps', 0)], writes=['ef'])
                    S.op('dve', lambda: nc.vector.tensor_scalar_add(T_['sgm'][:], T_['ef'][:], 1.0), reads=['ef'], writes=['sg0'])
                    S.op('dve', lambda: nc.vector.reciprocal(T_['sgm'][:], T_['sgm'][:]), reads=['sg0'], writes=['sgm'])
                    S.op('dve', lambda om1=om1, lb1=lb1: nc.vector.tensor_scalar(T_['lf'][:], T_['sgm'][:], om1, lb1, ALU.mult, ALU.add), reads=['sgm', 'const'], writes=['lf0'])
                    S.op('act', lambda: nc.scalar.activation(T_['lf'][:], T_['lf'][:], AF.Ln), reads=['lf0'], writes=['lf'])
                    S.op('dve', lambda om1=om1: nc.vector.scalar_tensor_tensor(T_['kin'][:], T_['ef'][:], om1, T_['sgm'][:], ALU.mult, ALU.mult), reads=['ef', 'sgm', 'const'], writes=['kin'])
                    S.op('dve', lambda: nc.vector.tensor_tensor_scan(T_['cum'][:], self.scanm[:], T_['lf'][:], 0.0, ALU.mult, ALU.add), reads=['lf', 'const'], writes=['cum'])
                    S.op('pool', lambda: nc.gpsimd.tensor_tensor(v3(T_['d1']), v3(T_['cum']), v3(T_['cum'])[:, :, 15:16].to_broadcast([128, 16, 32]), ALU.subtract), reads=['cum'], writes=['d1'])
                    S.op('act', lambda: nc.scalar.activation(T_['e1'][:], T_['d1'][:], AF.Exp), reads=['d1'], writes=['e1'])
                    S.op('act', lambda: nc.scalar.activation(T_['e2'][:], T_['d1'][:], AF.Exp, scale=-1.0), reads=['d1'], writes=['e2'])
                    S.op('act', lambda: nc.scalar.activation(T_['e3'][:], T_['cum'][:], AF.Exp), reads=['cum'], writes=['e3'])
                    S.op('pool', lambda: nc.gpsimd.tensor_tensor(v3(T_['d1']), v3(T_['cum'])[:, :, 31:32].to_broadcast([128, 16, 32]), v3(T_['cum']), ALU.subtract), reads=['cum', 'e1', 'e2', 'd1'], writes=['d1'])
                    S.op('act', lambda: nc.scalar.activation(T_['e4'][:], T_['d1'][:], AF.Exp), reads=['d1'], writes=['e4'])
                    S.op('act', lambda hd=hd: nc.scalar.activation(decs[:, hd, :], v3(T_['cum'])[:, :, 31], AF.Exp), reads=['cum'], writes=['decs'])
                    self.mm_group(self.bank(1), [(wb[:, kc, hd * 128:(hd + 1) * 128], h[:, kc, :]) for kc in range(8)], ['wb', hk], ('ps', 1))
                    S.op('dve', lambda hd=hd: nc.vector.tensor_tensor(q1s[:, hd, :], self.bank(1), T_['e1'][:], ALU.mult), reads=[('ps', 1), 'e1'], writes=['q1s'])
                    S.op('dve', lambda hd=hd: nc.vector.tensor_tensor(q2s[:, hd, :], self.bank(1), T_['e3'][:], ALU.mult), reads=[('ps', 1), 'e3'], writes=['q2s'])
                    S.op('pool', lambda hd=hd: nc.gpsimd.tensor_tensor(k1s[:, hd, :], T_['kin'][:], T_['e2'][:], ALU.mult), reads=['kin', 'e2'], writes=['k1s'])
                    S.op('pool', lambda: nc.gpsimd.tensor_tensor(ket[:], T_['kin'][:], T_['e4'][:], ALU.mult), reads=['kin', 'e4'], writes=['ket'])
                    for j in range(4):
                        self.mm_group(self.bank(2)[:, j * 128:(j + 1) * 128], [(ket[:, j * 128:(j + 1) * 128], self.ident[:])], ['ket', 'const'], ('ps', 2))
                    S.op('act', lambda hd=hd: nc.scalar.copy(kes[:, :, hd * 128:(hd + 1) * 128], self.bank(2).rearrange("p (j d) -> p j d", d=128)), reads=[('ps', 2)], writes=['kes'])
                S.dma('pool', self.fm(dr['BQ1'])[:, :, tsl], q1s[:], reads=['q1s'])
                S.dma('pool', self.fm(dr['BK1'])[:, :, tsl], k1s[:], reads=['k1s'])
                S.dma('pool', self.fm(dr['BQ2'])[:, :, tsl], q2s[:], reads=['q2s'])
                S.dma('pool', dr['BKE'][tsl, :].rearrange("(j p) f -> p j f", p=128), kes[:], reads=['kes'])
                S.dma('pool', dr['BDEC'][:, :, tt * 16:(tt + 1) * 16], decs[:], reads=['decs'])
                for sub in range(4):
                    for (c0, n, b) in ((0, 512, 4), (512, 256, 5)):
                        self.mm_group(self.bank(b, n), [(h[:, kc, sub * 128:(sub + 1) * 128], wb[:, kc, 2048 + c0:2048 + c0 + n]) for kc in range(8)], ['wb', hk], ('ps', b))
                        S.op('act', lambda sub=sub, c0=c0, n=n, b=b: nc.scalar.copy(bvs[:, sub, c0:c0 + n], self.bank(b, n)), reads=[('ps', b)], writes=['bvs'])
                    for (c0, n, b) in ((0, 512, 6), (512, 256, 7)):
                        self.mm_group(self.bank(b, n), [(h[:, kc, sub * 128:(sub + 1) * 128], wb[:, kc, 2816 + c0:2816 + c0 + n]) for kc in range(8)], ['wb', hk], ('ps', b))
                        S.op('act', lambda n=n, b=b: nc.scalar.activation(ge[:, 0:n], self.bank(b, n), AF.Exp, scale=-1.0), reads=[('ps', b)], writes=['ge'])
                        S.op('dve', lambda n=n: nc.vector.tensor_scalar_add(ge[:, 0:n], ge[:, 0:n], 1.0), reads=['ge'], writes=['ge'])
                        S.op('dve', lambda n=n: nc.vector.reciprocal(ge[:, 0:n], ge[:, 0:n]), reads=['ge'], writes=['ge'])
                        S.op('dve', lambda sub=sub, c0=c0, n=n, b=b: nc.vector.tensor_tensor(bgs[:, sub, c0:c0 + n], self.bank(b, n), ge[:, 0:n], ALU.mult), reads=['ge', ('ps', b)], writes=['bgs'])
                S.dma('pool', dr['BV'][tsl, :].rearrange("(j p) f -> p j f", p=128), bvs[:], reads=['bvs'])
                S.dma('pool', dr['BG'][tsl, :].rearrange("(j p) f -> p j f", p=128), bgs[:], reads=['bgs'])
        S.barrier()

    def p2c(self, l, HT, w_in, w_qb, w_kvb, g_q, g_kv):
        nc, S = self.nc, self.S
        dr = self.dram
        with contextlib.ExitStack() as st:
            wc = self.sb(st, "wc", [128, 8, 768], BF16)
            wqn = self.sb(st, "wqn", [128, 3, 768], BF16)
            wqr = self.sb(st, "wqr", [128, 3, 384], BF16)
            wkn = self.sb(st, "wkn", [128, 2, 768], BF16)
            wkv = self.sb(st, "wkv", [128, 2, 768], BF16)
            gq = self.sb(st, "gq", [128, 3], F32)
            gkv = self.sb(st, "gkv", [128, 2], F32)
            ht = [self.sb(st, "ht%d" % i, [128, 8, TS], BF16) for i in range(2)]
            tc_ = [self.sb(st, "tc%d" % i, [128, TS], F32) for i in range(2)]
            ts_ = [self.sb(st, "ts%d" % i, [128, TS], F32) for i in range(2)]
            cx = self.sb(st, "cx", [128, 5, TS], F32)
            cn = self.sb(st, "cn", [128, 5, TS], BF16)
            sq = self.sb(st, "sq", [128, 3, TS], BF16)
            rstd = self.sb(st, "rstd", [128, TS], F32)
            qb = self.sb(st, "qb", [128, TS], BF16)
            t1 = self.sb(st, "t1", [128, TS], F32)
            t2 = self.sb(st, "t2", [128, TS], F32)
            on = self.sb(st, "on", [128, 12, TS], BF16)
            orr = self.sb(st, "orr", [128, 4, TS], BF16)
            ov = self.sb(st, "ov", [128, 4, 768], BF16)
            S.dma('sp', gq[:], g_q, writes=['const'])
            S.dma('sp', gkv[:], g_kv, writes=['const'])
            self.load_w(wc, w_in, O_CQ, O_CQ + 704, 'wc', 8)
            self.load_w(wc, w_in, O_CKR, O_CKR + 64, 'wc', 8, dcol=704)
            for kc in range(3):
                rows = w_qb[kc * 128:(kc + 1) * 128, :].rearrange("p (h d) -> p h d", d=192)
                S.dma('pool', wqn[:, kc, :].rearrange("p (h d) -> p h d", d=128), rows[:, :, 0:128], writes=['wq'])
                S.dma('pool', wqr[:, kc, :].rearrange("p (h d) -> p h d", d=64), rows[:, :, 128:192], writes=['wq'])
            for kc in range(2):
                rows = w_kvb[kc * 128:(kc + 1) * 128, :].rearrange("p (h d) -> p h d", d=256)
                S.dma('pool', wkn[:, kc, :].rearrange("p (h d) -> p h d", d=128), rows[:, :, 0:128], writes=['wq'])
                S.dma('pool', wkv[:, kc, :].rearrange("p (h d) -> p h d", d=128), rows[:, :, 128:256], writes=['wq'])
            for tt in range(self.NT):
                i2 = tt % 2
                h, hk = ht[i2], 'ht%d' % i2
                tsl = slice(tt * TS, (tt + 1) * TS)
                psl = slice((tt % 8) * TS, (tt % 8 + 1) * TS)
                tk = 'tab%d' % i2
                S.dma('sp', h[:], self.fm(HT)[:, :, tsl], writes=[hk])
                S.dma('sp', tc_[i2][:], self.cin['ropeC_c'][:, psl], writes=[tk])
                S.dma('sp', ts_[i2][:], self.cin['ropeC_s'][:, psl], writes=[tk])
                for c in range(5):
                    b = c % 2
                    self.mm_group(self.bank(b), [(wc[:, kc, c * 128:(c + 1) * 128], h[:, kc, :]) for kc in range(8)], ['wc', hk], ('ps', b))
                    S.op('act', lambda c=c, b=b: nc.scalar.copy(cx[:, c, :], self.bank(b)), reads=[('ps', b)], writes=['cx'])
                self.mm_group(self.bank(2), [(wc[:, kc, 640:768], h[:, kc, :]) for kc in range(8)], ['wc', hk], ('ps', 2))
                self.rope(2, 3, tc_[i2][:], ts_[i2][:], self.permC, orr[:, 3, :], qb, t1, t2, tk, 'orr')
                self.rmsnorm(cx[:, 0:3, :], 3, 384, gq, cn[:, 0:3, :], ['cx'], 'cnq', sq[:], rstd[:], 'nq', 6)
                self.rmsnorm(cx[:, 3:5, :], 2, 256, gkv, cn[:, 3:5, :], ['cx'], 'cnk', sq[:, 0:2, :], rstd[:], 'nq', 6)
                for hd in range(6):
                    b = hd % 2
                    self.mm_group(self.bank(b), [(wqn[:, kc, hd * 128:(hd + 1) * 128], cn[:, kc, :]) for kc in range(3)], ['wq', 'cnq'], ('ps', b))
                    S.op('act', lambda hd=hd, b=b: nc.scalar.copy(on[:, hd, :], self.bank(b)), reads=[('ps', b)], writes=['on'])
                for j in range(3):
                    self.mm_group(self.bank(2), [(wqr[:, kc, j * 128:(j + 1) * 128], cn[:, kc, :]) for kc in range(3)], ['wq', 'cnq'], ('ps', 2))
                    self.rope(2, 3, tc_[i2][:], ts_[i2][:], self.permC, orr[:, j, :], qb, t1, t2, tk, 'orr')
                for hd in range(6):
                    b = hd % 2
                    self.mm_group(self.bank(b), [(wkn[:, kc, hd * 128:(hd + 1) * 128], cn[:, 3 + kc, :]) for kc in range(2)], ['wq', 'cnk'], ('ps', b))
                    S.op('act', lambda hd=hd, b=b: nc.scalar.copy(on[:, 6 + hd, :], self.bank(b)), reads=[('ps', b)], writes=['on'])
                for sub in range(4):
                    for (c0, n, b) in ((0, 512, 4), (512, 256, 5)):
                        self.mm_group(self.bank(b, n), [(cn[:, 3 + kc, sub * 128:(sub + 1) * 128], wkv[:, kc, c0:c0 + n]) for kc in range(2)], ['wq', 'cnk'], ('ps', b))
                        S.op('act', lambda sub=sub, c0=c0, n=n, b=b: nc.scalar.copy(ov[:, sub, c0:c0 + n], self.bank(b, n)), reads=[('ps', b)], writes=['ov'])
                S.dma('pool', self.fm(dr['CQN'])[:, :, tsl], on[:, 0:6, :], reads=['on'])
                S.dma('pool', self.fm(dr['CKN'])[:, :, tsl], on[:, 6:12, :], reads=['on'])
                S.dma('pool', self.fm(dr['CQR'])[:, :, tsl], orr[:, 0:3, :], reads=['orr'])
                S.dma('pool', dr['CKR'][:, tsl], orr[:, 3, :], reads=['orr'])
                S.dma('pool', dr['CV'][tsl, :].rearrange("(j p) f -> p j f", p=128), ov[:], reads=['ov'])
        S.barrier()

    def p3(self, l):
        nc, S = self.nc, self.S
        dr = self.dram
        with contextlib.ExitStack() as st:
            qn = self.sb(st, "qn", [128, 2, SEQ], BF16)
            kn = self.sb(st, "kn", [128, 2, SEQ], BF16)
            qp = self.sb(st, "qp", [128, 2, SEQ], BF16)
            kp = self.sb(st, "kp", [128, 2, SEQ], BF16)
            qm = [self.sb(st, "qm%d" % i, [128, 2, SEQ], BF16) for i in range(2)]
            for i in range(2):
                S.op('dve', lambda i=i: nc.vector.memset(qm[i][:], 0.0), writes=['qm'])
            vt = [self.sb(st, "vt%d" % i, [128, 4, 72], BF16) for i in range(2)]
            E = [self.sb(st, "E%d" % i, [128, 4, 256], BF16) for i in range(2)]
            ot = [self.sb(st, "ot%d" % i, [128, 260], F32) for i in range(2)]
            for i in range(2):
                S.op('dve', lambda i=i: nc.vector.memset(vt[i][:], 1.0), writes=['vt%d' % i])
            it = 0
            for s_ in range(self.nseq):
                for g, (window, dil) in enumerate(A_GROUPS):
                    if os.environ.get("P3G") and str(g) not in os.environ["P3G"]:
                        continue
                    n = SEQ // dil
                    nb = n // 128
                    t0 = s_ * SEQ
                    S.dma('sp', qn[:], self.fm(dr['AQ'])[:, 2 * g:2 * g + 2, t0:t0 + SEQ], writes=['qn'])
                    S.dma('sp', kn[:], self.fm(dr['AK'])[:, 2 * g:2 * g + 2, t0:t0 + SEQ], writes=['kn'])
                    for c in range(2):
                        S.op('dve', lambda c=c: nc.vector.tensor_copy(qm[0][0:64, c, :].rearrange("p (r j) -> p r j", r=dil), qn[0:64, c, :].rearrange("p (j r) -> p r j", r=dil)), reads=['qn'], writes=['qm'])
                        S.op('pool', lambda c=c: nc.gpsimd.tensor_copy(qm[1][64:128, c, :].rearrange("p (r j) -> p r j", r=dil), qn[64:128, c, :].rearrange("p (j r) -> p r j", r=dil)), reads=['qn'], writes=['qm'])
                    if dil > 1:
                        for c in range(2):
                            S.op('pool', lambda c=c: nc.gpsimd.tensor_copy(kp[:, c, :].rearrange("p (r j) -> p r j", r=dil), kn[:, c, :].rearrange("p (j r) -> p r j", r=dil)), reads=['kn'], writes=['kp'])
                        Kt, qk, kk_ = kp, 'qm', 'kp'
                    else:
                        Kt, qk, kk_ = kn, 'qm', 'kn'
                    for r in range(dil):
                        for qb in range(nb):
                            i2 = it % 2
                            it += 1
                            base = r * n + qb * 128
                            row0 = t0 + r + dil * 128 * qb
                            rows = slice(row0, row0 + dil * 127 + 1, dil)
                            vc, vck = vt[qb % 2], 'vt%d' % (qb % 2)
                            vp, vpk = vt[(qb + 1) % 2], 'vt%d' % ((qb + 1) % 2)
                            SK = os.environ.get('P3SKIP', '')
                            if 'v' not in SK:
                              S.dma('sp', vc[:, :, 0:64], dr['AV'][rows, 256 * g:256 * g + 256].rearrange("p (h d) -> p h d", d=64), writes=[vck])
                            bS = 2 * i2
                            for h4 in range(4):
                                c, po = h4 // 2, (h4 % 2) * 64
                                bk = bS + h4 // 2
                                off = (h4 % 2) * 256
                                if qb > 0 and 's' not in SK:
                                    self.mm_group(self.bank(bk)[:, off:off + 128], [(Kt[:, c, base - 128:base], qm[h4 % 2][:, c, base:base + 128])], [qk, kk_], ('ps', bk))
                                if 's' not in SK:
                                  self.mm_group(self.bank(bk)[:, off + 128:off + 256], [(Kt[:, c, base:base + 128], qm[h4 % 2][:, c, base:base + 128])], [qk, kk_], ('ps', bk))
                            Ek = 'E%d' % i2
                            for hb in range(2):
                                if 'e' not in SK:
                                  S.op('act', lambda i2=i2, bS=bS, hb=hb: nc.scalar.activation(E[i2][:, 2 * hb:2 * hb + 2, :].rearrange("p h q -> p (h q)"), self.bank(bS + hb), AF.Exp, scale=0.125),
                                     reads=[('ps', bS + hb)], writes=[Ek])
                            if 'm' not in SK:
                              S.op('dve', lambda i2=i2: nc.vector.tensor_tensor(E[i2][:], E[i2][:], self.mask4[:], ALU.mult), reads=[Ek, 'const2'], writes=[Ek + 'm'])
                            bO = 4 + i2
                            for h4 in range(4):
                                o_ = self.bank(bO)[:, h4 * 68:h4 * 68 + 65]
                                pairs = []
                                if qb > 0:
                                    pairs.append((E[i2][:, h4, 0:128], vp[:, h4, 0:65]))
                                pairs.append((E[i2][:, h4, 128:256], vc[:, h4, 0:65]))
                                if 'p' not in SK:
                                  self.mm_group(o_, pairs, [Ek + 'm', vck, vpk], ('ps', bO))
                            if 'o' not in SK:
                              S.op('dve', lambda i2=i2, bO=bO: nc.vector.tensor_copy(ot[i2][:].rearrange("p (h c) -> p h c", c=65), self.bank(bO)[:, 0:272].rearrange("p (h c) -> p h c", c=68)[:, :, 0:65]), reads=[('ps', bO)], writes=['ot%d' % i2])
                            if 'o' not in SK:
                              S.dma('pool', dr['AO'][g, rows, :], ot[i2][:], reads=['ot%d' % i2])
        S.barrier()

    def p4(self, l):
        nc, S = self.nc, self.S
        dr = self.dram
        with contextlib.ExitStack() as st:
            a = [self.sb(st, "a%d" % i, [128, 3, 260], F32) for i in range(2)]
            sm = self.sb(st, "sm", [128, 4, 65], F32)
            rd_ = self.sb(st, "rd", [128, 4, 1], F32)
            ya = self.sb(st, "ya", [128, 256], BF16)
            yat = [self.sb(st, "yat%d" % i, [128, 2, TS], BF16) for i in range(2)]
            for tt in range(self.NT):
                for sub in range(4):
                    blk = tt * 4 + sub
                    i2 = blk % 2
                    rows = slice(blk * 128, (blk + 1) * 128)
                    S.dma('sp', a[i2][:], dr['AO'][:, rows, :].rearrange("g p c -> p g c"), writes=['a%d' % i2])
                    smf = sm[:].rearrange("p h c -> p (h c)")
                    S.op('dve', lambda i2=i2: nc.vector.tensor_tensor(smf, a[i2][:, 0, :], a[i2][:, 1, :], ALU.add), reads=['a%d' % i2], writes=['sm0'])
                    S.op('dve', lambda i2=i2: nc.vector.tensor_tensor(smf, smf, a[i2][:, 2, :], ALU.add), reads=['a%d' % i2, 'sm0'], writes=['sm'])
                    S.op('dve', lambda: nc.vector.reciprocal(rd_[:], sm[:, :, 64:65]), reads=['sm'], writes=['rd'])
                    S.op('dve', lambda: nc.vector.tensor_tensor(ya[:].rearrange("p (h d) -> p h d", d=64), sm[:, :, 0:64], rd_[:].to_broadcast([128, 4, 64]), ALU.mult), reads=['sm', 'rd'], writes=['ya'])
                    for c in range(2):
                        self.mm_group(self.bank(c)[:, sub * 128:(sub + 1) * 128], [(ya[:, c * 128:(c + 1) * 128], self.ident[:])], ['ya', 'const'], ('ps', c))
                j2 = tt % 2
                for c in range(2):
                    S.op('act', lambda c=c, j2=j2: nc.scalar.copy(yat[j2][:, c, :], self.bank(c)), reads=[('ps', c)], writes=['yat%d' % j2])
                S.dma('pool', self.fm(dr['YA'])[:, :, tt * TS:(tt + 1) * TS], yat[j2][:], reads=['yat%d' % j2])
        S.barrier()

    def p5(self, l):
        nc, S = self.nc, self.S
        dr = self.dram
        scale = 192.0 ** -0.5
        with contextlib.ExitStack() as st:
            qn = [self.sb(st, "qn%d" % i, [128, SEQ], BF16) for i in range(2)]
            qr = [self.sb(st, "qr%d" % i, [128, SEQ], BF16) for i in range(2)]
            for i in range(2):
                S.op('dve', lambda i=i: nc.vector.memset(qr[i][:], 0.0), writes=['qr%d' % i])
            kn = [self.sb(st, "kn%d" % i, [128, SEQ], BF16) for i in range(2)]
            kr = self.sb(st, "kr", [128, SEQ], BF16)
            vt = [self.sb(st, "vt%d" % i, [128, 32, 136], BF16) for i in range(2)]
            E = [self.sb(st, "E%d" % i, [128, TS], BF16) for i in range(2)]
            rec = self.sb(st, "rec", [128, 4], F32)
            ob = [self.sb(st, "ob%d" % i, [128, 128], BF16) for i in range(2)]
            yc = [self.sb(st, "yc%d" % i, [128, TS], BF16) for i in range(2)]
            for i in range(2):
                S.op('dve', lambda i=i: nc.vector.memset(vt[i][:], 1.0), writes=['vt%d' % i])
            it = 0
            hh = 0
            for s_ in range(self.nseq):
                t0 = s_ * SEQ
                S.dma('sp', kr[:], dr['CKR'][:, t0:t0 + SEQ], writes=['kr'])
                for hd in range(6):
                    b2 = hh % 2
                    hh += 1
                    po = (hd % 2) * 64
                    keys = ['qn%d' % b2, 'qr%d' % b2, 'kn%d' % b2, 'vt%d' % b2, 'kr']
                    S.dma('sp', qn[b2][:], dr['CQN'][hd * 128:(hd + 1) * 128, t0:t0 + SEQ], writes=[keys[0]])
                    S.dma('sp', qr[b2][po:po + 64, :], dr['CQR'][(hd // 2) * 128 + po:(hd // 2) * 128 + po + 64, t0:t0 + SEQ], writes=[keys[1]])
                    S.dma('sp', kn[b2][:], dr['CKN'][hd * 128:(hd + 1) * 128, t0:t0 + SEQ], writes=[keys[2]])
                    S.dma('sp', vt[b2][:, :, 0:128], dr['CV'][t0:t0 + SEQ, hd * 128:(hd + 1) * 128].rearrange("(b p) d -> p b d", p=128), writes=[keys[3]])
                    its = [(qt_, kb_) for qt_ in range(8) for kb_ in range(4 * qt_ + 4)]
                    it0 = it
                    it += len(its)

                    def geom(idx):
                        qt_, kb_ = its[idx]
                        i2_ = (it0 + idx) % 2
                        j0_ = max(0, kb_ - 4 * qt_)
                        return qt_, kb_, i2_, j0_, TS - 128 * j0_, slice(qt_ * TS + 128 * j0_, (qt_ + 1) * TS), slice(kb_ * 128, (kb_ + 1) * 128), 4 + i2_

                    def emit_S(idx):
                        qt_, kb_, i2_, j0_, ncol_, qsl_, ksl_, bS_ = geom(idx)
                        self.mm_group(self.bank(bS_, ncol_), [(kn[b2][:, ksl_], qn[b2][:, qsl_]), (kr[:, ksl_], qr[b2][:, qsl_])], keys, ('ps', bS_))
                    emit_S(0)
                    for idx in range(len(its)):
                        qt, kb, i2, j0, ncol, qsl, ksl, bS = geom(idx)
                        if idx + 1 < len(its):
                            emit_S(idx + 1)
                        Ek = 'E%d' % i2
                        S.op('act', lambda i2=i2, bS=bS, ncol=ncol: nc.scalar.activation(E[i2][:, 0:ncol], self.bank(bS, ncol), AF.Exp, scale=scale), reads=[('ps', bS)], writes=[Ek])
                        if kb >= 4 * qt:
                            S.op('dve', lambda i2=i2: nc.vector.tensor_tensor(E[i2][:, 0:128], E[i2][:, 0:128], self.mcur[:], ALU.mult), reads=[Ek, 'const'], writes=[Ek])
                        for j in range(j0, 4):
                            S.op('pe', lambda j=j, i2=i2, kb=kb, j0=j0, qt=qt: nc.tensor.matmul(self.bank(j, 129), E[i2][:, (j - j0) * 128:(j - j0 + 1) * 128], vt[b2][:, kb, 0:129], start=(kb == 0), stop=(kb == 4 * qt + j)),
                                 reads=[Ek] + keys, writes=[('ps', j)], signal=(kb == 4 * qt + j))
                        if kb != 4 * qt + 3:
                            continue
                        y2 = qt % 2
                        for j in range(4):
                            S.op('dve', lambda j=j: nc.vector.reciprocal(rec[:, j:j + 1], self.bank(j)[:, 128:129]), reads=[('ps', j)], writes=['rec'])
                        for j in range(4):
                            o2 = j % 2
                            S.op('dve', lambda j=j, o2=o2: nc.vector.tensor_scalar(ob[o2][:], self.bank(j, 128), rec[:, j:j + 1], None, ALU.mult), reads=[('ps', j), 'rec'], writes=['ob%d' % o2])
                            self.mm_group(self.bank(6)[:, j * 128:(j + 1) * 128], [(ob[o2][:], self.ident[:])], ['ob%d' % o2, 'const'], ('ps', 6))
                        S.op('act', lambda y2=y2: nc.scalar.copy(yc[y2][:], self.bank(6)), reads=[('ps', 6)], writes=['yc%d' % y2])
                        S.dma('pool', dr['YC'][hd * 128:(hd + 1) * 128, t0 + qt * TS:t0 + (qt + 1) * TS], yc[y2][:], reads=['yc%d' % y2])
        S.barrier()

    def p6(self, l, g_b):
        nc, S = self.nc, self.S
        dr = self.dram
        TL = 256
        NCH = TL // 32

        def oreg(bank0, hd, p):
            b = bank0 + (1 if hd >= 5 else 0)
            o = (hd % 5) * 96 if hd < 5 else (hd - 5) * 96
            return self.ps[0:p, b * 512 + o:b * 512 + o + 96], b
        with contextlib.ExitStack() as st:
            q1 = [self.sb(st, "q1%d" % i, [128, 8, TL], BF16) for i in range(2)]
            k1 = [self.sb(st, "k1%d" % i, [128, 8, TL], BF16) for i in range(2)]
            q2 = [self.sb(st, "q2%d" % i, [128, 8, TL], BF16) for i in range(2)]
            ke = [self.sb(st, "ke%d" % i, [32, NCH, 1024], BF16) for i in range(2)]
            bv = [self.sb(st, "bv%d" % i, [32, NCH, 768], BF16) for i in range(2)]
            bg = [self.sb(st, "bg%d" % i, [32, NCH, 768], BF16) for i in range(2)]
            dec = [self.sb(st, "dec%d" % i, [128, 8, NCH], F32) for i in range(2)]
            gn = self.sb(st, "gn", [32, 768], F32)
            stf = self.sb(st, "stf", [128, 8, 96], F32)
            stb = self.sb(st, "stb", [128, 8, 96], BF16)
            aT = [self.sb(st, "aT%d" % i, [32, 8, 32], BF16) for i in range(2)]
            osb = self.sb(st, "osb", [32, NCH, 768], F32)
            sq = self.sb(st, "sq", [32, NCH, 768], F32)
            ss = self.sb(st, "ss", [32, NCH * 8], F32)
            yb = self.sb(st, "yb", [32, NCH, 768], BF16)
            ybt = [self.sb(st, "ybt%d" % i, [128, 6, TL], BF16) for i in range(2)]
            S.dma('sp', gn[:], g_b, writes=['const'])
            ti = 0
            for s_ in range(self.nseq):
                S.op('dve', lambda: nc.vector.memset(stf[:], 0.0), reads=['stb'], writes=['stf'])
                S.op('pool', lambda: nc.gpsimd.memset(stb[:], 0.0), writes=['stb'])
                for tl in range(SEQ // TL):
                    i2 = ti % 2
                    ti += 1
                    t0 = s_ * SEQ + tl * TL
                    tsl = slice(t0, t0 + TL)
                    kq = ['q1%d' % i2, 'k1%d' % i2, 'q2%d' % i2, 'ke%d' % i2, 'bv%d' % i2, 'bg%d' % i2, 'dec%d' % i2]
                    S.dma('sp', q1[i2][:], self.fm(dr['BQ1'])[:, :, tsl], writes=[kq[0]])
                    S.dma('sp', k1[i2][:], self.fm(dr['BK1'])[:, :, tsl], writes=[kq[1]])
                    S.dma('sp', q2[i2][:], self.fm(dr['BQ2'])[:, :, tsl], writes=[kq[2]])
                    S.dma('sp', ke[i2][:], dr['BKE'][tsl, :].rearrange("(c p) f -> p c f", p=32), writes=[kq[3]])
                    S.dma('sp', bv[i2][:], dr['BV'][tsl, :].rearrange("(c p) f -> p c f", p=32), writes=[kq[4]])
                    S.dma('sp', bg[i2][:], dr['BG'][tsl, :].rearrange("(c p) f -> p c f", p=32), writes=[kq[5]])
                    S.dma('sp', dec[i2][:], dr['BDEC'][:, :, t0 // 32:t0 // 32 + NCH], writes=[kq[6]])
                    for c in range(NCH):
                        cs = slice(c * 32, (c + 1) * 32)
                        a2 = c % 2
                        for hd in range(8):
                            self.mm_group(self.ps[0:32, hd * 32:(hd + 1) * 32], [(k1[i2][:, hd, cs], q1[i2][:, hd, cs])], [kq[0], kq[1]], ('ps', 0))
                        S.op('dve', lambda a2=a2: nc.vector.tensor_tensor(aT[a2][:], self.ps[0:32, 0:256].rearrange("p (h l) -> p h l", l=32), self.mask8[:], ALU.mult), reads=[('ps', 0), 'const2'], writes=['aT%d' % a2])
                        for hd in range(8):
                            o_, b = oreg(1, hd, 32)
                            self.mm_group(o_, [(aT[a2][:, hd, :], bv[i2][:, c, hd * 96:(hd + 1) * 96]), (q2[i2][:, hd, cs], stb[:, hd, :])], ['aT%d' % a2, kq[4], kq[2], 'stb'], ('ps', b))
                        for hd in range(8):
                            u_, b = oreg(3, hd, 128)
                            self.mm_group(u_, [(ke[i2][:, c, hd * 128:(hd + 1) * 128], bv[i2][:, c, hd * 96:(hd + 1) * 96])], [kq[3], kq[4]], ('ps', b))
                        S.op('act', lambda c=c: nc.scalar.copy(osb[:, c, 0:480], self.ps[0:32, 512:512 + 480]), reads=[('ps', 1)], writes=['osb'])
                        S.op('act', lambda c=c: nc.scalar.copy(osb[:, c, 480:768], self.ps[0:32, 1024:1024 + 288]), reads=[('ps', 2)], writes=['osb'])
                        for hd in range(8):
                            u_, b = oreg(3, hd, 128)
                            S.op('dve', lambda hd=hd, u_=u_, c=c: nc.vector.scalar_tensor_tensor(stf[:, hd, :], stf[:, hd, :], dec[i2][:, hd, c:c + 1], u_, ALU.mult, ALU.add),
                                 reads=[('ps', b), kq[6], 'stf'], writes=['stf'])
                        S.op('act', lambda: nc.scalar.copy(stb[:], stf[:]), reads=['stf'], writes=['stb'])
                    o4 = osb[:].rearrange("p c (h d) -> p (c h) d", d=96)
                    S.op('pool', lambda: nc.gpsimd.tensor_tensor(sq[:], osb[:], osb[:], ALU.mult), reads=['osb'], writes=['sq'])
                    S.op('dve', lambda: nc.vector.tensor_reduce(ss[:], sq[:].rearrange("p c (h d) -> p (c h) d", d=96), AX.X, ALU.add), reads=['sq'], writes=['ss0'])
                    S.op('dve', lambda: nc.vector.tensor_scalar(ss[:], ss[:], 1.0 / 96, EPS, ALU.mult, ALU.add), reads=['ss0'], writes=['ss1'])
                    S.op('act', lambda: nc.scalar.activation(ss[:], ss[:], AF.Sqrt), reads=['ss1'], writes=['ss2'])
                    S.op('dve', lambda: nc.vector.reciprocal(ss[:], ss[:]), reads=['ss2'], writes=['ss'])
                    S.op('pool', lambda: nc.gpsimd.tensor_tensor(sq[:].rearrange("p c (h d) -> p (c h) d", d=96), o4, ss[:].unsqueeze(2).to_broadcast([32, NCH * 8, 96]), ALU.mult), reads=['osb', 'ss'], writes=['sq'])
                    S.op('pool', lambda: nc.gpsimd.tensor_tensor(sq[:], sq[:], gn[:].unsqueeze(1).to_broadcast([32, NCH, 768]), ALU.mult), reads=['sq', 'const'], writes=['sq'])
                    S.op('pool', lambda: nc.gpsimd.tensor_tensor(yb[:], sq[:], bg[i2][:], ALU.mult), reads=['sq', kq[5]], writes=['yb'])
                    for c in range(NCH):
                        for f in range(6):
                            b = 5 + (f // 4)
                            self.mm_group(self.ps[:, b * 512 + (f % 4) * 128 + 0:b * 512 + (f % 4) * 128 + 32], [(yb[:, c, f * 128:(f + 1) * 128], self.ident[0:32, 0:32])], ['yb', 'const'], ('ps', b))
                        S.op('act', lambda c=c: nc.scalar.copy(ybt[i2][:, 0:4, c * 32:(c + 1) * 32], self.ps[:, 5 * 512:6 * 512].rearrange("p (f t) -> p f t", t=128)[:, :, 0:32]), reads=[('ps', 5)], writes=['ybt%d' % i2])
                        S.op('act', lambda c=c: nc.scalar.copy(ybt[i2][:, 4:6, c * 32:(c + 1) * 32], self.ps[:, 6 * 512:6 * 512 + 256].rearrange("p (f t) -> p f t", t=128)[:, :, 0:32]), reads=[('ps', 6)], writes=['ybt%d' % i2])
                    S.dma('pool', self.fm(dr['YB'])[:, :, tsl], ybt[i2][:], reads=['ybt%d' % i2])
        S.barrier()

    def p7(self, l, XT, HT, w_in, w_a, w_b, w_c, w_o):
        nc, S = self.nc, self.S
        dr = self.dram
        with contextlib.ExitStack() as st:
            wg = self.sb(st, "wg", [128, 8, 3072], BF16)
            wa = self.sb(st, "wa", [128, 2, D], BF16)
            wb = self.sb(st, "wb", [128, 6, D], BF16)
            wc = self.sb(st, "wc", [128, 6, D], BF16)
            wo = self.sb(st, "wo", [128, 8, D], BF16)
            ht = [self.sb(st, "ht%d" % i, [128, 8, TS], BF16) for i in range(2)]
            yin = [self.sb(st, "yin%d" % i, [128, 14, TS], BF16) for i in range(2)]
            xt = [self.sb(st, "xt%d" % i, [128, 8, TS], F32) for i in range(2)]
            gt = [self.sb(st, "gt%d" % i, [128, TS], F32) for i in range(2)]
            acc = self.sb(st, "acc", [128, TS], F32)
            tmp = self.sb(st, "tmp", [128, TS], F32)
            mg = self.sb(st, "mg", [128, 8, TS], BF16)
            self.load_w(wg, w_in, O_G, O_G + 3072, 'wg', 8)
            self.load_w(wa, w_a, 0, D, 'wa', 2)
            self.load_w(wb, w_b, 0, D, 'wb', 6)
            self.load_w(wc, w_c, 0, D, 'wc', 6)
            self.load_w(wo, w_o, 0, D, 'wo', 8)
            ysrc = ((dr['YA'], 0, 2, wa), (dr['YB'], 2, 6, wb), (dr['YC'], 8, 6, wc))
            for tt in range(self.NT):
                i2 = tt % 2
                tsl = slice(tt * TS, (tt + 1) * TS)
                h, hk = ht[i2], 'ht%d' % i2
                y, yk = yin[i2], 'yin%d' % i2
                x, xk = xt[i2], 'xt%d' % i2
                S.dma('sp', h[:], self.fm(HT)[:, :, tsl], writes=[hk])
                for (src, o0, n, _) in ysrc:
                    S.dma('sp', y[:, o0:o0 + n, :], self.fm(src)[:, :, tsl], writes=[yk])
                S.dma('sp', x[:], self.fm(XT)[:, :, tsl], writes=[xk])
                for dc in range(8):
                    for bi, (src, o0, n, w_) in enumerate(ysrc):
                        bgt = (dc * 3 + bi) % 2
                        g_ = gt[bgt]
                        gk = 'gt%d' % bgt
                        bG, bP = 2 * bgt, 2 * bgt + 1
                        self.mm_group(self.bank(bG), [(wg[:, kc, bi * D + dc * 128:bi * D + (dc + 1) * 128], h[:, kc, :]) for kc in range(8)], ['wg', hk], ('ps', bG))
                        self.mm_group(self.bank(bP), [(w_[:, kc, dc * 128:(dc + 1) * 128], y[:, o0 + kc, :]) for kc in range(n)], ['wa', 'wb', 'wc', yk], ('ps', bP))
                        S.op('act', lambda g_=g_, bG=bG: nc.scalar.activation(g_[:], self.bank(bG), AF.Sigmoid), reads=[('ps', bG)], writes=[gk])
                        if bi == 0:
                            S.op('dve', lambda g_=g_, bP=bP: nc.vector.tensor_tensor(acc[:], g_[:], self.bank(bP), ALU.mult), reads=[gk, ('ps', bP)], writes=['acc'])
                        else:
                            S.op('dve', lambda g_=g_, bP=bP: nc.vector.tensor_tensor(tmp[:], g_[:], self.bank(bP), ALU.mult), reads=[gk, ('ps', bP)], writes=['tmp'])
                            if bi == 1:
                                S.op('dve', lambda: nc.vector.tensor_tensor(acc[:], acc[:], tmp[:], ALU.add), reads=['acc', 'tmp'], writes=['acc'])
                            else:
                                S.op('dve', lambda dc=dc: nc.vector.tensor_tensor(mg[:, dc, :], acc[:], tmp[:], ALU.add), reads=['acc', 'tmp'], writes=['mg'])
                for dc in range(8):
                    b = 4 + dc % 2
                    self.mm_group(self.bank(b), [(wo[:, kc, dc * 128:(dc + 1) * 128], mg[:, kc, :]) for kc in range(8)], ['wo', 'mg'], ('ps', b))
                    S.op('dve', lambda dc=dc, b=b, x=x: nc.vector.tensor_tensor(x[:, dc, :], x[:, dc, :], self.bank(b), ALU.add), reads=[('ps', b), xk], writes=[xk])
                S.dma('pool', self.fm(XT)[:, :, tsl], x[:], reads=[xk])
        S.barrier()

    def finish(self):
        self.S.barrier()
        self.es.close()


def make_consts():
    import ml_dtypes
    bf = ml_dtypes.bfloat16
    c = {}
    c['ident'] = np.eye(128, dtype=np.float32)
    k = np.arange(128)[:, None]
    q = np.arange(128)[None, :]
    c['mcur'] = (k <= q).astype(np.float32)
    c['mprev'] = (k >= q).astype(np.float32)
    pa = np.zeros((128, 128), np.float32)
    pc = np.zeros((128, 128), np.float32)
    for m in range(128):
        i = m % 64
        if i < 8:
            pa[m + 8, m] = -1.0
        elif i < 16:
            pa[m - 8, m] = 1.0
        if i < 32:
            pc[m + 32, m] = -1.0
        else:
            pc[m - 32, m] = 1.0
    c['permA'] = pa
    c['permC'] = pc
    sm = np.ones((128, TS), np.float32)
    sm[:, ::32] = 0.0
    c['scanm'] = sm
    t = np.arange(SEQ, dtype=np.float32)
    inv_a = (1.0 / (np.float32(500000.0) ** (np.arange(0, 16, 2, dtype=np.float32) / np.float32(16)))).astype(np.float32)
    ang_a = t[:, None] * inv_a[None, :]
    ca = np.ones((128, SEQ), np.float32)
    sa = np.zeros((128, SEQ), np.float32)
    inv_c = (1.0 / (np.float32(10000.0) ** (np.arange(0, 64, 2, dtype=np.float32) / np.float32(64)))).astype(np.float32)
    ang_c = t[:, None] * inv_c[None, :]
    cc = np.zeros((128, SEQ), np.float32)
    sc = np.zeros((128, SEQ), np.float32)
    for m in range(128):
        i = m % 64
        if i < 16:
            ca[m] = np.cos(ang_a[:, i % 8])
            sa[m] = np.sin(ang_a[:, i % 8])
        cc[m] = np.cos(ang_c[:, i % 32])
        sc[m] = np.sin(ang_c[:, i % 32])
    c['ropeA_c'], c['ropeA_s'], c['ropeC_c'], c['ropeC_s'] = ca, sa, cc, sc
    return {k_: np.ascontiguousarray(v.astype(np.float32)) for k_, v in c.items()}


CONST_SHAPES = {'ident': [128, 128], 'mcur': [128, 128], 'mprev': [128, 128], 'permA': [128, 128], 'permC': [128, 128],
                'scanm': [128, TS], 'ropeA_c': [128, SEQ], 'ropeA_s': [128, SEQ], 'ropeC_c': [128, SEQ], 'ropeC_s': [128, SEQ]}

W_SHAPES = {
    'w_ffn1_up': [D, 2 * DFF], 'w_ffn1_down': [DFF, D], 'w_in': [D, 9664], 'w_c_qb': [384, 1152], 'w_c_kvb': [256, 1536],
    'w_branch_a': [256, D], 'w_branch_b': [768, D], 'w_branch_c': [768, D], 'w_out': [D, D],
    'w_ffn2_up': [D, 2 * DFF], 'w_ffn2_down': [DFF, D], 'w_ple_gate': [D, D], 'w_ple_proj': [256, D],
}
V_SHAPES = {
    'norm_ffn1': [128, 8], 'norm_mix': [128, 8], 'norm_ffn2': [128, 8], 'norm_ple': [128, 8],
    'c_q_norm': [128, 3], 'c_kv_norm': [128, 2], 'b_gnorm': [32, 768],
}


def host_layout(inputs, core, nseq, depth):
    m = {}
    xb = inputs['x'][core * nseq:(core + 1) * nseq]
    m['xT'] = np.ascontiguousarray(xb.reshape(nseq * SEQ, D).T)
    pb = inputs['p'][:depth, core * nseq:(core + 1) * nseq]
    m['pT'] = np.ascontiguousarray(pb.reshape(depth, nseq * SEQ, 256).transpose(0, 2, 1))
    for k_ in W_SHAPES:
        m[k_] = np.ascontiguousarray(inputs[k_][:depth])
    for k_ in ('norm_ffn1', 'norm_mix', 'norm_ffn2', 'norm_ple', 'c_q_norm', 'c_kv_norm'):
        v = inputs[k_][:depth]
        m[k_] = np.ascontiguousarray(v.reshape(depth, -1, 128).transpose(0, 2, 1))
    m['norm_final'] = np.ascontiguousarray(inputs['norm_final'].reshape(8, 128).T)
    g = inputs['b_gnorm'][:depth]
    m['b_gnorm'] = np.ascontiguousarray(np.broadcast_to(g[:, None, None, :], (depth, 32, 8, 96)).reshape(depth, 32, 768))
    lbl = inputs['b_lb_logits']
    m['b_lb_logits'] = np.ascontiguousarray(lbl.reshape(lbl.shape[0], 8, 128).transpose(2, 1, 0))
    return m


def build(nseq=2, depth=DEPTH, dump=(), upto=99, consts_only=False):
    nc = bass.Bass("TRN2", target_bir_lowering=False)
    kb = K(nc, nseq, depth, dump)
    T = kb.T
    ein = lambda name, shape, dt=F32: nc.dram_tensor(name, list(shape), dt, kind="ExternalInput").ap()
    cin = {k_: ein(k_, s) for k_, s in CONST_SHAPES.items()}
    xT = ein('xT', [D, T])
    pT = ein('pT', [depth, 256, T])
    W = {k_: ein(k_, [depth] + s) for k_, s in W_SHAPES.items()}
    V = {k_: ein(k_, [depth] + s) for k_, s in V_SHAPES.items()}
    V['norm_final'] = ein('norm_final', [128, 8])
    lbl = ein('b_lb_logits', [128, 8, DEPTH])
    outT = nc.dram_tensor('outT', [D, T], F32, kind="ExternalOutput").ap()
    kb.cin = cin
    kb.lbl = lbl
    kb.setup_consts(cin)
    XT = kb.dt('XT', [D, T], F32)
    HT = kb.dt('HT', [D, T], BF16)
    kb.alloc_scratch()
    kb.S.barrier()
    for l in range(depth):
        last = (l == depth - 1)
        kb.phase_ffn(l, xT if l == 0 else XT, XT, W['w_ffn1_up'][l], W['w_ffn1_down'][l], V['norm_ffn1'][l], V['norm_mix'][l], HT)
        if upto >= 2:
            kb.phase_mixer(l, XT, HT, W, V, lbl)
        if upto >= 3:
            kb.phase_ffn(l, XT, XT, W['w_ffn2_up'][l], W['w_ffn2_down'][l], V['norm_ffn2'][l])
        kb.phase_ple(l, XT, XT, pT[l], W['w_ple_gate'][l], W['w_ple_proj'][l], V['norm_ple'][l],
                     V['norm_final'] if last else None, outT if last else None)
    kb.finish()
    return nc, kb


_CONSTS = None


def kernel(**inputs):
    global _CONSTS
    inputs = {k_: np.asarray(v) for k_, v in inputs.items()}
    if _CONSTS is None:
        _CONSTS = make_consts()
    nseq = 2
    nc, kb = build(nseq=nseq, depth=DEPTH)
    in_maps = []
    for c in range(NCORES):
        m = host_layout(inputs, c, nseq, DEPTH)
        m.update(_CONSTS)
        in_maps.append(m)
    res = run_bass_kernel_spmd(nc, in_maps, core_ids=list(range(NCORES)))
    outs = []
    for c in range(NCORES):
        o = res.results[c]['outT']
        outs.append(np.ascontiguousarray(o.T).reshape(nseq, SEQ, D))
    return np.concatenate(outs, axis=0).astype(np.float32)
```

```python
import contextlib
import os
import numpy as np
import concourse.bass as bass
import concourse.mybir as mybir
from concourse.bass_utils import run_bass_kernel_spmd

F32 = mybir.dt.float32
BF16 = mybir.dt.bfloat16
ALU = mybir.AluOpType
AF = mybir.ActivationFunctionType
AX = mybir.AxisListType

D = 1024
SEQ = 4096
DEPTH = 4
DFF = 2816
EPS = 1e-6
NCORES = 8
TS = 512
O_AQ, O_AK, O_AV, O_BQ, O_BF, O_BI, O_BG, O_CQ, O_CKV, O_CKR, O_G = 0, 768, 1536, 2304, 3328, 4352, 5120, 5888, 6272, 6528, 6592
A_GROUPS = ((128, 1), (512, 4), (2048, 16))


class Sched:
    EPOCH = 30000
    NDS = 16

    def __init__(self, nc, es):
        self.nc = nc
        self.es = es
        self.E = {'pe': nc.tensor, 'act': nc.scalar, 'dve': nc.vector, 'pool': nc.gpsimd, 'sp': nc.sync}
        self.cnt = {e: 0 for e in ('pe', 'act', 'dve', 'pool')}
        self.csem = {e: [] for e in self.cnt}
        self.dsem = [es.enter_context(nc.semaphore("d%d" % i)) for i in range(self.NDS)]
        self.dval = [0] * self.NDS
        self.ndma = 0
        self.wc = {q: {} for q in self.E}
        self.wd = {q: {} for q in self.E}
        self.lw = {}
        self.rd = {}
        self.groups = {}
        self.ninst = 0

    def _csem(self, e, idx):
        ep = (idx - 1) // self.EPOCH
        while len(self.csem[e]) <= ep:
            self.csem[e].append(self.es.enter_context(self.nc.semaphore("c_%s_%d" % (e, len(self.csem[e])))))
        return self.csem[e][ep], idx - ep * self.EPOCH

    def _wait(self, q, tok):
        if tok[0] == 'c':
            _, e, idx = tok
            if e == q and e == 'pe':
                return
            if self.wc[q].get(e, 0) >= idx:
                return
            assert idx <= self.cnt[e], "wait on unsignaled instr %s %s" % (q, tok)
            s, v = self._csem(e, idx)
            self.E[q].wait_ge(s, v)
            self.wc[q][e] = idx
        else:
            _, j, v = tok
            if self.wd[q].get(j, 0) >= v:
                return
            self.E[q].wait_ge(self.dsem[j], v)
            self.wd[q][j] = v
        self.ninst += 1

    def _deps(self, q, reads, writes, is_dma):
        for k in reads:
            for kk in self.groups.get(k, (k,)):
                t = self.lw.get(kk)
                if t is not None:
                    self._wait(q, t)
        for k in writes:
            t = self.lw.get(k)
            if t is not None and (is_dma or not (t[0] == 'c' and t[1] == q)):
                self._wait(q, t)
            for t2 in self.rd.get(k, {}).values():
                if is_dma or not (t2[0] == 'c' and t2[1] == q):
                    self._wait(q, t2)

    def op(self, e, fn, reads=(), writes=(), signal=True):
        psr = [k for k in reads if isinstance(k, tuple) and k[0] == 'ps']
        if psr:
            reads = [k for k in reads if k not in psr]
            writes = list(writes) + psr
        self._deps(e, reads, writes, False)
        ins = fn()
        self.ninst += 1
        if signal:
            self.cnt[e] += 1
            idx = self.cnt[e]
            s, v = self._csem(e, idx)
            ins.then_inc(s, 1)
        else:
            idx = self.cnt[e] + 1
        tok = ('c', e, idx)
        for k in reads:
            self.rd.setdefault(k, {})[e] = tok
        for k in writes:
            self.lw[k] = tok
            self.rd[k] = {}
        return ins

    def dma(self, q, out, in_, reads=(), writes=()):
        i = self.ndma
        self.ndma += 1
        j = i % self.NDS
        if self.dval[j] > 0:
            self._wait(q, ('d', j, self.dval[j]))
        self._deps(q, reads, writes, True)
        self.dval[j] += 16
        self.E[q].dma_start(out=out, in_=in_).then_inc(self.dsem[j], 16)
        self.ninst += 1
        tok = ('d', j, self.dval[j])
        for k in reads:
            self.rd.setdefault(k, {})[('d', j)] = tok
        for k in writes:
            self.lw[k] = tok
            self.rd[k] = {}

    def barrier(self):
        for q in self.E:
            for e in self.cnt:
                if e != q and self.cnt[e] > 0:
                    self._wait(q, ('c', e, self.cnt[e]))
            for j in range(self.NDS):
                if self.dval[j] > 0:
                    self._wait(q, ('d', j, self.dval[j]))
        self.lw = {}
        self.rd = {}
        self.groups = {}


class K:
    def __init__(self, nc, nseq, depth, dump=()):
        self.nc = nc
        self.nseq = nseq
        self.T = nseq * SEQ
        self.NT = self.T // TS
        self.depth = depth
        self.dump = set(dump)
        self.es = contextlib.ExitStack()
        self.S = Sched(nc, self.es)
        self.ps = self.es.enter_context(nc.psum_tensor("ps", [128, 4096], F32))
        self.dram = {}

    def bank(self, b, n=512, p=128):
        return self.ps[0:p, b * 512:b * 512 + n]

    def dt(self, name, shape, dtype, kind=None):
        if kind is None:
            kind = "ExternalOutput" if name in self.dump else "Internal"
        t = self.nc.dram_tensor(name, list(shape), dtype, kind=kind).ap()
        self.dram[name] = t
        return t

    def sb(self, st, name, shape, dtype):
        self.uid = getattr(self, 'uid', 0) + 1
        return st.enter_context(self.nc.sbuf_tensor("s%d_%s" % (self.uid, name), list(shape), dtype))

    def mm_group(self, out, pairs, reads, wkey):
        n = len(pairs)
        for i, (l, r) in enumerate(pairs):
            self.S.op('pe', lambda l=l, r=r, i=i: self.nc.tensor.matmul(out, l, r, start=(i == 0), stop=(i == n - 1)),
                      reads=reads, writes=[wkey], signal=(i == n - 1))

    def load_w(self, dst, src_rows, c0, c1, key, kc_n, dcol=0):
        for kc in range(kc_n):
            self.S.dma('pool', dst[:, kc, dcol:dcol + (c1 - c0)], src_rows[kc * 128:(kc + 1) * 128, c0:c1], writes=[(key, kc, dcol)])
        self.S.groups[key] = list(self.S.groups.get(key, [])) + [(key, kc, dcol) for kc in range(kc_n)]

    def rmsnorm(self, x, KC, Dn, g, out, keys_in, key_out, sq, rstd, kp, pb):
        nc, S = self.nc, self.S
        N = x.shape[-1]
        S.op('act', lambda: nc.scalar.activation(sq, x, AF.Square), reads=keys_in, writes=[kp + 'sq'])
        bk = self.bank(pb, N)
        self.mm_group(bk, [(self.ones[:], sq[:, kc, :]) for kc in range(KC)], [kp + 'sq', 'const'], ('ps', pb))
        S.op('dve', lambda: nc.vector.tensor_scalar(rstd, bk, 1.0 / Dn, EPS, ALU.mult, ALU.add), reads=[('ps', pb)], writes=[kp + 'rs0'])
        S.op('act', lambda: nc.scalar.activation(rstd, rstd, AF.Sqrt), reads=[kp + 'rs0'], writes=[kp + 'rs1'])
        S.op('dve', lambda: nc.vector.reciprocal(rstd, rstd), reads=[kp + 'rs1'], writes=[kp + 'rstd'])
        for kc in range(KC):
            S.op('dve', lambda kc=kc: nc.vector.scalar_tensor_tensor(out[:, kc, :], x[:, kc, :], g[:, kc:kc + 1], rstd, ALU.mult, ALU.mult),
                 reads=keys_in + [kp + 'rstd', 'const'], writes=[key_out])

    def setup_consts(self, cin):
        nc, S = self.nc, self.S
        st = self.es
        self.ones = self.sb(st, "ones", [128, 128], BF16)
        self.ident = self.sb(st, "ident", [128, 128], BF16)
        self.mcur = self.sb(st, "mcur", [128, 128], BF16)
        self.mprev = self.sb(st, "mprev", [128, 128], BF16)
        self.permA = self.sb(st, "permA", [128, 128], BF16)
        self.permC = self.sb(st, "permC", [128, 128], BF16)
        self.scanm = self.sb(st, "scanm", [128, TS], F32)
        S.op('dve', lambda: nc.vector.memset(self.ones[:], 1.0), writes=['const'])
        for t, nm in ((self.ident, 'ident'), (self.mcur, 'mcur'), (self.mprev, 'mprev'), (self.permA, 'permA'), (self.permC, 'permC')):
            S.dma('pool', t[:], cin[nm], writes=['const'])
        S.dma('sp', self.scanm[:], cin['scanm'], writes=['const'])

    def phase_ffn(self, l, src, dst, w_up, w_dn, g_ffn, g_post=None, ht_dst=None):
        nc, S = self.nc, self.S
        with contextlib.ExitStack() as st:
            wup = self.sb(st, "wup", [128, 8, 2 * DFF], BF16)
            wdn = self.sb(st, "wdn", [128, 22, D], BF16)
            gf = self.sb(st, "gf", [128, 8], F32)
            gp = self.sb(st, "gp", [128, 8], F32)
            xt = [self.sb(st, "xt%d" % i, [128, 8, TS], F32) for i in range(2)]
            ht = self.sb(st, "ht", [128, 8, TS], BF16)
            at = self.sb(st, "at", [128, 22, TS], BF16)
            sg = [self.sb(st, "sg%d" % i, [128, TS], F32) for i in range(2)]
            rstd = self.sb(st, "rstd", [128, TS], F32)
            S.dma('sp', gf[:], g_ffn, writes=['const'])
            if g_post is not None:
                S.dma('sp', gp[:], g_post, writes=['const'])
            for half in range(2):
                self.load_w(wup, w_up, half * DFF, (half + 1) * DFF, 'wup', 8, dcol=half * DFF)
            self.load_w(wdn, w_dn, 0, D, 'wdn', 22)
            srcv = src.rearrange("(c p) t -> p c t", p=128)
            dstv = dst.rearrange("(c p) t -> p c t", p=128)
            for tt in range(self.NT):
                x = xt[tt % 2]
                xk = 'xt%d' % (tt % 2)
                tsl = slice(tt * TS, (tt + 1) * TS)
                S.dma('sp', x[:], srcv[:, :, tsl], writes=[xk])
                self.rmsnorm(x[:], 8, D, gf, ht[:], [xk], 'ht', at[:, 0:8, :], rstd[:], 'n1', 6)
                for j in range(22):
                    bg, bu = 2 * (j % 2), 2 * (j % 2) + 1
                    self.mm_group(self.bank(bg), [(wup[:, kc, j * 128:(j + 1) * 128], ht[:, kc, :]) for kc in range(8)], ['wup', 'ht'], ('ps', bg))
                    self.mm_group(self.bank(bu), [(wup[:, kc, DFF + j * 128:DFF + (j + 1) * 128], ht[:, kc, :]) for kc in range(8)], ['wup', 'ht'], ('ps', bu))
                    s_ = sg[j % 2]
                    S.op('act', lambda s_=s_, bg=bg: nc.scalar.activation(s_[:], self.bank(bg), AF.Silu), reads=[('ps', bg)], writes=['sg%d' % (j % 2)])
                    S.op('dve', lambda s_=s_, bu=bu, j=j: nc.vector.tensor_tensor(at[:, j, :], s_[:], self.bank(bu), ALU.mult),
                         reads=['sg%d' % (j % 2), ('ps', bu)], writes=['n1sq' if j < 8 else 'at'])
                for dc in range(8):
                    b = 4 + dc % 2
                    self.mm_group(self.bank(b), [(wdn[:, kc, dc * 128:(dc + 1) * 128], at[:, kc, :]) for kc in range(22)], ['wdn', 'at', 'n1sq'], ('ps', b))
                    S.op('dve', lambda dc=dc, b=b: nc.vector.scalar_tensor_tensor(x[:, dc, :], self.bank(b), 0.5, x[:, dc, :], ALU.mult, ALU.add),
                         reads=[('ps', b), xk], writes=[xk])
                S.dma('pool', dstv[:, :, tsl], x[:], reads=[xk])
                if g_post is not None:
                    self.rmsnorm(x[:], 8, D, gp, ht[:], [xk], 'ht', at[:, 0:8, :], rstd[:], 'n1', 6)
                    S.dma('pool', ht_dst.rearrange("(c p) t -> p c t", p=128)[:, :, tsl], ht[:], reads=['ht'])
        S.barrier()

    def phase_ple(self, l, src, dst, pT, w_pg, w_pp, g_ple, g_fin=None, out_dst=None):
        nc, S = self.nc, self.S
        with contextlib.ExitStack() as st:
            wpg = self.sb(st, "wpg", [128, 8, D], BF16)
            wpp = self.sb(st, "wpp", [128, 2, D], BF16)
            gl = self.sb(st, "gl", [128, 8], F32)
            gfn = self.sb(st, "gfn", [128, 8], F32)
            xt = [self.sb(st, "xt%d" % i, [128, 8, TS], F32) for i in range(2)]
            pt = [self.sb(st, "pt%d" % i, [128, 2, TS], BF16) for i in range(2)]
            ht = self.sb(st, "ht", [128, 8, TS], BF16)
            sq = self.sb(st, "sq", [128, 8, TS], BF16)
            xo = self.sb(st, "xo", [128, 8, TS], F32)
            sg = [self.sb(st, "sg%d" % i, [128, TS], F32) for i in range(2)]
            rstd = self.sb(st, "rstd", [128, TS], F32)
            S.dma('sp', gl[:], g_ple, writes=['const'])
            if g_fin is not None:
                S.dma('sp', gfn[:], g_fin, writes=['const'])
            self.load_w(wpg, w_pg, 0, D, 'wpg', 8)
            self.load_w(wpp, w_pp, 0, D, 'wpp', 2)
            srcv = src.rearrange("(c p) t -> p c t", p=128)
            dstv = dst.rearrange("(c p) t -> p c t", p=128)
            pv = pT.rearrange("(c p) t -> p c t", p=128)
            for tt in range(self.NT):
                x = xt[tt % 2]
                xk = 'xt%d' % (tt % 2)
                p_ = pt[tt % 2]
                pk = 'pt%d' % (tt % 2)
                tsl = slice(tt * TS, (tt + 1) * TS)
                S.dma('sp', x[:], srcv[:, :, tsl], writes=[xk])
                S.dma('pool', p_[:], pv[:, :, tsl], writes=[pk])
                self.rmsnorm(x[:], 8, D, gl, ht[:], [xk], 'ht', sq[:], rstd[:], 'n1', 6)
                for dc in range(8):
                    b0, b1 = 2 * (dc % 2), 2 * (dc % 2) + 1
                    self.mm_group(self.bank(b0), [(wpg[:, kc, dc * 128:(dc + 1) * 128], ht[:, kc, :]) for kc in range(8)], ['wpg', 'ht'], ('ps', b0))
                    self.mm_group(self.bank(b1), [(wpp[:, kc, dc * 128:(dc + 1) * 128], p_[:, kc, :]) for kc in range(2)], ['wpp', pk], ('ps', b1))
                    s_ = sg[dc % 2]
                    sk = 'sg%d' % (dc % 2)
                    S.op('act', lambda s_=s_, b0=b0: nc.scalar.activation(s_[:], self.bank(b0), AF.Sigmoid), reads=[('ps', b0)], writes=[sk])
                    S.op('dve', lambda s_=s_, b1=b1: nc.vector.tensor_tensor(s_[:], s_[:], self.bank(b1), ALU.mult), reads=[sk, ('ps', b1)], writes=[sk])
                    S.op('dve', lambda s_=s_, dc=dc: nc.vector.tensor_tensor(x[:, dc, :], x[:, dc, :], s_[:], ALU.add), reads=[sk, xk], writes=[xk])
                if g_fin is None:
                    S.dma('pool', dstv[:, :, tsl], x[:], reads=[xk])
                else:
                    self.rmsnorm(x[:], 8, D, gfn, xo[:], [xk], 'xo', sq[:], rstd[:], 'n1', 6)
                    S.dma('pool', out_dst.rearrange("(c p) t -> p c t", p=128)[:, :, tsl], xo[:], reads=['xo'])
        S.barrier()

    def alloc_scratch(self):
        T = self.T
        for nm, shp, dt_ in (
            ('AQ', [768, T], BF16), ('AK', [768, T], BF16), ('AV', [T, 768], BF16), ('AO', [3, T, 260], F32), ('YA', [256, T], BF16),
            ('BQ1', [1024, T], BF16), ('BK1', [1024, T], BF16), ('BQ2', [1024, T], BF16), ('BKE', [T, 1024], BF16),
            ('BDEC', [128, 8, T // 32], F32), ('BV', [T, 768], BF16), ('BG', [T, 768], BF16), ('YB', [768, T], BF16),
            ('CQN', [768, T], BF16), ('CQR', [384, T], BF16), ('CKN', [768, T], BF16), ('CKR', [128, T], BF16),
            ('CV', [T, 768], BF16), ('YC', [768, T], BF16)):
            self.dt(nm, shp, dt_)
        nc, S, st = self.nc, self.S, self.es
        self.lbc = self.sb(st, "lbc", [128, 8, DEPTH], F32)
        self.oml = self.sb(st, "oml", [128, 8, DEPTH], F32)
        ex = self.sb(st, "lbex", [128, 8, DEPTH], F32)
        ss = self.sb(st, "lbss", [128, 8], F32)
        self.mask4 = self.sb(st, "mask4", [128, 4, 256], BF16)
        self.mask8 = self.sb(st, "mask8", [32, 8, 32], BF16)
        S.dma('sp', ex[:], self.lbl, writes=['lbex'])
        S.op('act', lambda: nc.scalar.activation(ex[:], ex[:], AF.Exp), reads=['lbex'], writes=['lbex'])
        S.op('dve', lambda: nc.vector.tensor_reduce(ss[:], ex[:], AX.X, ALU.add), reads=['lbex'], writes=['lbss'])
        S.op('dve', lambda: nc.vector.reciprocal(ss[:], ss[:]), reads=['lbss'], writes=['lbss2'])
        S.op('dve', lambda: nc.vector.tensor_tensor(ex[:], ex[:], ss[:].unsqueeze(2).to_broadcast([128, 8, DEPTH]), ALU.mult), reads=['lbss2', 'lbex'], writes=['lbp'])
        S.op('dve', lambda: nc.vector.memset(self.lbc[:, :, 0:1], 0.0), writes=['lbc0'])
        S.op('dve', lambda: nc.vector.tensor_copy(self.lbc[:, :, 1:2], ex[:, :, 1:2]), reads=['lbp'], writes=['lbc1'])
        for i in (2, 3):
            S.op('dve', lambda i=i: nc.vector.tensor_tensor(self.lbc[:, :, i:i + 1], self.lbc[:, :, i - 1:i], ex[:, :, i:i + 1], ALU.add),
                 reads=['lbp', 'lbc%d' % (i - 1)], writes=['lbc%d' % i])
        S.op('dve', lambda: nc.vector.tensor_scalar(self.oml[:], self.lbc[:], -1.0, 1.0, ALU.mult, ALU.add), reads=['lbc3', 'lbc0', 'lbc1', 'lbc2'], writes=['const'])
        for h4 in range(4):
            S.op('pool', lambda h4=h4: nc.gpsimd.tensor_copy(self.mask4[:, h4, 0:128], self.mprev[:]), reads=['const'], writes=['const2'])
            S.op('pool', lambda h4=h4: nc.gpsimd.tensor_copy(self.mask4[:, h4, 128:256], self.mcur[:]), reads=['const'], writes=['const2'])
        for h in range(8):
            S.op('pool', lambda h=h: nc.gpsimd.tensor_copy(self.mask8[:, h, :], self.mcur[0:32, 0:32]), reads=['const'], writes=['const2'])

    def rope(self, bq, bp, tc, ts, perm, out, qb, t1, t2, kq, okey):
        nc, S = self.nc, self.S
        S.op('act', lambda: nc.scalar.copy(qb[:], self.bank(bq)), reads=[('ps', bq)], writes=['qb'])
        self.mm_group(self.bank(bp), [(perm[:], qb[:])], ['qb', 'const'], ('ps', bp))
        S.op('dve', lambda: nc.vector.tensor_tensor(t1[:], self.bank(bq), tc, ALU.mult), reads=[('ps', bq), kq], writes=['t1'])
        S.op('dve', lambda: nc.vector.tensor_tensor(t2[:], self.bank(bp), ts, ALU.mult), reads=[('ps', bp), kq], writes=['t2'])
        S.op('pool', lambda: nc.gpsimd.tensor_tensor(out, t1[:], t2[:], ALU.add), reads=['t1', 't2'], writes=[okey])

    def fm(self, ap2d):
        return ap2d.rearrange("(c p) t -> p c t", p=128)

    def phase_mixer(self, l, XT, HT, W, V, lbl):
        import os
        ph = os.environ.get('MIXPH', 'a,b,c,3,4,5,6,7').split(',')
        if 'a' in ph:
            self.p2a(l, HT, W['w_in'][l])
        if 'b' in ph:
            self.p2b(l, HT, W['w_in'][l])
        if 'c' in ph:
            self.p2c(l, HT, W['w_in'][l], W['w_c_qb'][l], W['w_c_kvb'][l], V['c_q_norm'][l], V['c_kv_norm'][l])
        if '3' in ph:
            self.p3(l)
        if '4' in ph:
            self.p4(l)
        if '5' in ph:
            self.p5(l)
        if '6' in ph:
            self.p6(l, V['b_gnorm'][l])
        if '7' in ph:
            self.p7(l, XT, HT, W['w_in'][l], W['w_branch_a'][l], W['w_branch_b'][l], W['w_branch_c'][l], W['w_out'][l])

    def p2a(self, l, HT, w_in):
        nc, S = self.nc, self.S
        dr = self.dram
        with contextlib.ExitStack() as st:
            wa = self.sb(st, "wa", [128, 8, 2304], BF16)
            ht = [self.sb(st, "ht%d" % i, [128, 8, TS], BF16) for i in range(2)]
            tc_ = [self.sb(st, "tc%d" % i, [128, TS], F32) for i in range(2)]
            ts_ = [self.sb(st, "ts%d" % i, [128, TS], F32) for i in range(2)]
            qb = self.sb(st, "qb", [128, TS], BF16)
            t1 = self.sb(st, "t1", [128, TS], F32)
            t2 = self.sb(st, "t2", [128, TS], F32)
            oq = [self.sb(st, "oq%d" % i, [128, 12, TS], BF16) for i in range(2)]
            ov = [self.sb(st, "ov%d" % i, [128, 4, 768], BF16) for i in range(2)]
            self.load_w(wa, w_in, 0, 2304, 'wa', 8)
            for tt in range(self.NT):
                i2 = tt % 2
                h, hk = ht[i2], 'ht%d' % i2
                tsl = slice(tt * TS, (tt + 1) * TS)
                psl = slice((tt % 8) * TS, (tt % 8 + 1) * TS)
                S.dma('sp', h[:], self.fm(HT)[:, :, tsl], writes=[hk])
                S.dma('sp', tc_[i2][:], self.cin['ropeA_c'][:, psl], writes=['tab%d' % i2])
                S.dma('sp', ts_[i2][:], self.cin['ropeA_s'][:, psl], writes=['tab%d' % i2])
                for c in range(12):
                    bq, bp = 2 * (c % 2), 2 * (c % 2) + 1
                    self.mm_group(self.bank(bq), [(wa[:, kc, c * 128:(c + 1) * 128], h[:, kc, :]) for kc in range(8)], ['wa', hk], ('ps', bq))
                    self.rope(bq, bp, tc_[i2][:], ts_[i2][:], self.permA, oq[i2][:, c, :], qb, t1, t2, 'tab%d' % i2, 'oq%d' % i2)
                S.dma('pool', self.fm(dr['AQ'])[:, :, tsl], oq[i2][:, 0:6, :], reads=['oq%d' % i2])
                S.dma('pool', self.fm(dr['AK'])[:, :, tsl], oq[i2][:, 6:12, :], reads=['oq%d' % i2])
                for sub in range(4):
                    for (c0, n, b) in ((0, 512, 4), (512, 256, 5)):
                        self.mm_group(self.bank(b, n), [(h[:, kc, sub * 128:(sub + 1) * 128], wa[:, kc, 1536 + c0:1536 + c0 + n]) for kc in range(8)], ['wa', hk], ('ps', b))
                        S.op('act', lambda sub=sub, c0=c0, n=n, b=b: nc.scalar.copy(ov[i2][:, sub, c0:c0 + n], self.bank(b, n)), reads=[('ps', b)], writes=['ov%d' % i2])
                S.dma('pool', dr['AV'][tsl, :].rearrange("(j p) f -> p j f", p=128), ov[i2][:], reads=['ov%d' % i2])
        S.barrier()

    def p2b(self, l, HT, w_in):
        nc, S = self.nc, self.S
        dr = self.dram
        with contextlib.ExitStack() as st:
            wb = self.sb(st, "wb", [128, 8, 3584], BF16)
            ht = [self.sb(st, "ht%d" % i, [128, 8, TS], BF16) for i in range(2)]
            f32t = {n: self.sb(st, n, [128, TS], F32) for n in ('ef', 'sgm', 'kin', 'lf', 'cum', 'd1', 'e1', 'e2', 'e3', 'e4')}
            q1s = self.sb(st, "q1s", [128, 8, TS], BF16)
            k1s = self.sb(st, "k1s", [128, 8, TS], BF16)
            q2s = self.sb(st, "q2s", [128, 8, TS], BF16)
            ket = self.sb(st, "ket", [128, TS], BF16)
            kes = self.sb(st, "kes", [128, 4, 1024], BF16)
            decs = self.sb(st, "decs", [128, 8, 16], F32)
            bvs = self.sb(st, "bvs", [128, 4, 768], BF16)
            bgs = self.sb(st, "bgs", [128, 4, 768], BF16)
            ge = self.sb(st, "ge", [128, 512], F32)
            self.load_w(wb, w_in, O_BQ, O_BQ + 3584, 'wb', 8)
            T_ = f32t
            v3 = lambda t: t[:].rearrange("p (c l) -> p c l", l=32)
            for tt in range(self.NT):
                i2 = tt % 2
                h, hk = ht[i2], 'ht%d' % i2
                tsl = slice(tt * TS, (tt + 1) * TS)
                S.dma('sp', h[:], self.fm(HT)[:, :, tsl], writes=[hk])
                for hd in range(8):
                    lb1 = self.lbc[:, hd, l:l + 1]
                    om1 = self.oml[:, hd, l:l + 1]
                    self.mm_group(self.bank(0), [(wb[:, kc, 1024 + hd * 128:1024 + (hd + 1) * 128], h[:, kc, :]) for kc in range(8)], ['wb', hk], ('ps', 0))
                    S.op('act', lambda: nc.scalar.activation(T_['ef'][:], self.bank(0), AF.Exp, scale=-1.0), reads=[('ps', 0)], writes=['ef'])
                    S.op('dve', lambda: nc.vector.tensor_scalar_add(T_['sgm'][:], T_['ef'][:], 1.0), reads=['ef'], writes=['sg0'])
                    S.op('dve', lambda: nc.vector.reciprocal(T_['sgm'][:], T_['sgm'][:]), reads=['sg0'], writes=['sgm'])
                    S.op('dve', lambda om1=om1, lb1=lb1: nc.vector.tensor_scalar(T_['lf'][:], T_['sgm'][:], om1, lb1, ALU.mult, ALU.add), reads=['sgm', 'const'], writes=['lf0'])
                    S.op('act', lambda: nc.scalar.activation(T_['lf'][:], T_['lf'][:], AF.Ln), reads=['lf0'], writes=['lf'])
                    S.op('dve', lambda om1=om1: nc.vector.scalar_tensor_tensor(T_['kin'][:], T_['ef'][:], om1, T_['sgm'][:], ALU.mult, ALU.mult), reads=['ef', 'sgm', 'const'], writes=['kin'])
                    S.op('dve', lambda: nc.vector.tensor_tensor_scan(T_['cum'][:], self.scanm[:], T_['lf'][:], 0.0, ALU.mult, ALU.add), reads=['lf', 'const'], writes=['cum'])
                    S.op('pool', lambda: nc.gpsimd.tensor_tensor(v3(T_['d1']), v3(T_['cum']), v3(T_['cum'])[:, :, 15:16].to_broadcast([128, 16, 32]), ALU.subtract), reads=['cum'], writes=['d1'])
                    S.op('act', lambda: nc.scalar.activation(T_['e1'][:], T_['d1'][:], AF.Exp), reads=['d1'], writes=['e1'])
                    S.op('act', lambda: nc.scalar.activation(T_['e2'][:], T_['d1'][:], AF.Exp, scale=-1.0), reads=['d1'], writes=['e2'])
                    S.op('act', lambda: nc.scalar.activation(T_['e3'][:], T_['cum'][:], AF.Exp), reads=['cum'], writes=['e3'])
                    S.op('pool', lambda: nc.gpsimd.tensor_tensor(v3(T_['d1']), v3(T_['cum'])[:, :, 31:32].to_broadcast([128, 16, 32]), v3(T_['cum']), ALU.subtract), reads=['cum', 'e1', 'e2', 'd1'], writes=['d1'])
                    S.op('act', lambda: nc.scalar.activation(T_['e4'][:], T_['d1'][:], AF.Exp), reads=['d1'], writes=['e4'])
                    S.op('act', lambda hd=hd: nc.scalar.activation(decs[:, hd, :], v3(T_['cum'])[:, :, 31], AF.Exp), reads=['cum'], writes=['decs'])
                    self.mm_group(self.bank(1), [(wb[:, kc, hd * 128:(hd + 1) * 128], h[:, kc, :]) for kc in range(8)], ['wb', hk], ('ps', 1))
                    S.op('dve', lambda hd=hd: nc.vector.tensor_tensor(q1s[:, hd, :], self.bank(1), T_['e1'][:], ALU.mult), reads=[('ps', 1), 'e1'], writes=['q1s'])
                    S.op('dve', lambda hd=hd: nc.vector.tensor_tensor(q2s[:, hd, :], self.bank(1), T_['e3'][:], ALU.mult), reads=[('ps', 1), 'e3'], writes=['q2s'])
                    S.op('pool', lambda hd=hd: nc.gpsimd.tensor_tensor(k1s[:, hd, :], T_['kin'][:], T_['e2'][:], ALU.mult), reads=['kin', 'e2'], writes=['k1s'])
                    S.op('pool', lambda: nc.gpsimd.tensor_tensor(ket[:], T_['kin'][:], T_['e4'][:], ALU.mult), reads=['kin', 'e4'], writes=['ket'])
                    for j in range(4):
                        self.mm_group(self.bank(2)[:, j * 128:(j + 1) * 128], [(ket[:, j * 128:(j + 1) * 128], self.ident[:])], ['ket', 'const'], ('ps', 2))
                    S.op('act', lambda hd=hd: nc.scalar.copy(kes[:, :, hd * 128:(hd + 1) * 128], self.bank(2).rearrange("p (j d) -> p j d", d=128)), reads=[('ps', 2)], writes=['kes'])
                S.dma('pool', self.fm(dr['BQ1'])[:, :, tsl], q1s[:], reads=['q1s'])
                S.dma('pool', self.fm(dr['BK1'])[:, :, tsl], k1s[:], reads=['k1s'])
                S.dma('pool', self.fm(dr['BQ2'])[:, :, tsl], q2s[:], reads=['q2s'])
                S.dma('pool', dr['BKE'][tsl, :].rearrange("(j p) f -> p j f", p=128), kes[:], reads=['kes'])
                S.dma('pool', dr['BDEC'][:, :, tt * 16:(tt + 1) * 16], decs[:], reads=['decs'])
                for sub in range(4):
                    for (c0, n, b) in ((0, 512, 4), (512, 256, 5)):
                        self.mm_group(self.bank(b, n), [(h[:, kc, sub * 128:(sub + 1) * 128], wb[:, kc, 2048 + c0:2048 + c0 + n]) for kc in range(8)], ['wb', hk], ('ps', b))
                        S.op('act', lambda sub=sub, c0=c0, n=n, b=b: nc.scalar.copy(bvs[:, sub, c0:c0 + n], self.bank(b, n)), reads=[('ps', b)], writes=['bvs'])
                    for (c0, n, b) in ((0, 512, 6), (512, 256, 7)):
                        self.mm_group(self.bank(b, n), [(h[:, kc, sub * 128:(sub + 1) * 128], wb[:, kc, 2816 + c0:2816 + c0 + n]) for kc in range(8)], ['wb', hk], ('ps', b))
                        S.op('act', lambda n=n, b=b: nc.scalar.activation(ge[:, 0:n], self.bank(b, n), AF.Exp, scale=-1.0), reads=[('ps', b)], writes=['ge'])
                        S.op('dve', lambda n=n: nc.vector.tensor_scalar_add(ge[:, 0:n], ge[:, 0:n], 1.0), reads=['ge'], writes=['ge'])
                        S.op('dve', lambda n=n: nc.vector.reciprocal(ge[:, 0:n], ge[:, 0:n]), reads=['ge'], writes=['ge'])
                        S.op('dve', lambda sub=sub, c0=c0, n=n, b=b: nc.vector.tensor_tensor(bgs[:, sub, c0:c0 + n], self.bank(b, n), ge[:, 0:n], ALU.mult), reads=['ge', ('ps', b)], writes=['bgs'])
                S.dma('pool', dr['BV'][tsl, :].rearrange("(j p) f -> p j f", p=128), bvs[:], reads=['bvs'])
                S.dma('pool', dr['BG'][tsl, :].rearrange("(j p) f -> p j f", p=128), bgs[:], reads=['bgs'])
        S.barrier()

    def p2c(self, l, HT, w_in, w_qb, w_kvb, g_q, g_kv):
        nc, S = self.nc, self.S
        dr = self.dram
        with contextlib.ExitStack() as st:
            wc = self.sb(st, "wc", [128, 8, 768], BF16)
            wqn = self.sb(st, "wqn", [128, 3, 768], BF16)
            wqr = self.sb(st, "wqr", [128, 3, 384], BF16)
            wkn = self.sb(st, "wkn", [128, 2, 768], BF16)
            wkv = self.sb(st, "wkv", [128, 2, 768], BF16)
            gq = self.sb(st, "gq", [128, 3], F32)
            gkv = self.sb(st, "gkv", [128, 2], F32)
            ht = [self.sb(st, "ht%d" % i, [128, 8, TS], BF16) for i in range(2)]
            tc_ = [self.sb(st, "tc%d" % i, [128, TS], F32) for i in range(2)]
            ts_ = [self.sb(st, "ts%d" % i, [128, TS], F32) for i in range(2)]
            cx = self.sb(st, "cx", [128, 5, TS], F32)
            cn = self.sb(st, "cn", [128, 5, TS], BF16)
            sq = self.sb(st, "sq", [128, 3, TS], BF16)
            rstd = self.sb(st, "rstd", [128, TS], F32)
            qb = self.sb(st, "qb", [128, TS], BF16)
            t1 = self.sb(st, "t1", [128, TS], F32)
            t2 = self.sb(st, "t2", [128, TS], F32)
            on = self.sb(st, "on", [128, 12, TS], BF16)
            orr = self.sb(st, "orr", [128, 4, TS], BF16)
            ov = self.sb(st, "ov", [128, 4, 768], BF16)
            S.dma('sp', gq[:], g_q, writes=['const'])
            S.dma('sp', gkv[:], g_kv, writes=['const'])
            self.load_w(wc, w_in, O_CQ, O_CQ + 704, 'wc', 8)
            self.load_w(wc, w_in, O_CKR, O_CKR + 64, 'wc', 8, dcol=704)
            for kc in range(3):
                rows = w_qb[kc * 128:(kc + 1) * 128, :].rearrange("p (h d) -> p h d", d=192)
                S.dma('pool', wqn[:, kc, :].rearrange("p (h d) -> p h d", d=128), rows[:, :, 0:128], writes=['wq'])
                S.dma('pool', wqr[:, kc, :].rearrange("p (h d) -> p h d", d=64), rows[:, :, 128:192], writes=['wq'])
            for kc in range(2):
                rows = w_kvb[kc * 128:(kc + 1) * 128, :].rearrange("p (h d) -> p h d", d=256)
                S.dma('pool', wkn[:, kc, :].rearrange("p (h d) -> p h d", d=128), rows[:, :, 0:128], writes=['wq'])
                S.dma('pool', wkv[:, kc, :].rearrange("p (h d) -> p h d", d=128), rows[:, :, 128:256], writes=['wq'])
            for tt in range(self.NT):
                i2 = tt % 2
                h, hk = ht[i2], 'ht%d' % i2
                tsl = slice(tt * TS, (tt + 1) * TS)
                psl = slice((tt % 8) * TS, (tt % 8 + 1) * TS)
                tk = 'tab%d' % i2
                S.dma('sp', h[:], self.fm(HT)[:, :, tsl], writes=[hk])
                S.dma('sp', tc_[i2][:], self.cin['ropeC_c'][:, psl], writes=[tk])
                S.dma('sp', ts_[i2][:], self.cin['ropeC_s'][:, psl], writes=[tk])
                for c in range(5):
                    b = c % 2
                    self.mm_group(self.bank(b), [(wc[:, kc, c * 128:(c + 1) * 128], h[:, kc, :]) for kc in range(8)], ['wc', hk], ('ps', b))
                    S.op('act', lambda c=c, b=b: nc.scalar.copy(cx[:, c, :], self.bank(b)), reads=[('ps', b)], writes=['cx'])
                self.mm_group(self.bank(2), [(wc[:, kc, 640:768], h[:, kc, :]) for kc in range(8)], ['wc', hk], ('ps', 2))
                self.rope(2, 3, tc_[i2][:], ts_[i2][:], self.permC, orr[:, 3, :], qb, t1, t2, tk, 'orr')
                self.rmsnorm(cx[:, 0:3, :], 3, 384, gq, cn[:, 0:3, :], ['cx'], 'cnq', sq[:], rstd[:], 'nq', 6)
                self.rmsnorm(cx[:, 3:5, :], 2, 256, gkv, cn[:, 3:5, :], ['cx'], 'cnk', sq[:, 0:2, :], rstd[:], 'nq', 6)
                for hd in range(6):
                    b = hd % 2
                    self.mm_group(self.bank(b), [(wqn[:, kc, hd * 128:(hd + 1) * 128], cn[:, kc, :]) for kc in range(3)], ['wq', 'cnq'], ('ps', b))
                    S.op('act', lambda hd=hd, b=b: nc.scalar.copy(on[:, hd, :], self.bank(b)), reads=[('ps', b)], writes=['on'])
                for j in range(3):
                    self.mm_group(self.bank(2), [(wqr[:, kc, j * 128:(j + 1) * 128], cn[:, kc, :]) for kc in range(3)], ['wq', 'cnq'], ('ps', 2))
                    self.rope(2, 3, tc_[i2][:], ts_[i2][:], self.permC, orr[:, j, :], qb, t1, t2, tk, 'orr')
                for hd in range(6):
                    b = hd % 2
                    self.mm_group(self.bank(b), [(wkn[:, kc, hd * 128:(hd + 1) * 128], cn[:, 3 + kc, :]) for kc in range(2)], ['wq', 'cnk'], ('ps', b))
                    S.op('act', lambda hd=hd, b=b: nc.scalar.copy(on[:, 6 + hd, :], self.bank(b)), reads=[('ps', b)], writes=['on'])
                for sub in range(4):
                    for (c0, n, b) in ((0, 512, 4), (512, 256, 5)):
                        self.mm_group(self.bank(b, n), [(cn[:, 3 + kc, sub * 128:(sub + 1) * 128], wkv[:, kc, c0:c0 + n]) for kc in range(2)], ['wq', 'cnk'], ('ps', b))
                        S.op('act', lambda sub=sub, c0=c0, n=n, b=b: nc.scalar.copy(ov[:, sub, c0:c0 + n], self.bank(b, n)), reads=[('ps', b)], writes=['ov'])
                S.dma('pool', self.fm(dr['CQN'])[:, :, tsl], on[:, 0:6, :], reads=['on'])
                S.dma('pool', self.fm(dr['CKN'])[:, :, tsl], on[:, 6:12, :], reads=['on'])
                S.dma('pool', self.fm(dr['CQR'])[:, :, tsl], orr[:, 0:3, :], reads=['orr'])
                S.dma('pool', dr['CKR'][:, tsl], orr[:, 3, :], reads=['orr'])
                S.dma('pool', dr['CV'][tsl, :].rearrange("(j p) f -> p j f", p=128), ov[:], reads=['ov'])
        S.barrier()

    def p3(self, l):
        nc, S = self.nc, self.S
        dr = self.dram
        with contextlib.ExitStack() as st:
            qn = self.sb(st, "qn", [128, 2, SEQ], BF16)
            kn = self.sb(st, "kn", [128, 2, SEQ], BF16)
            qp = self.sb(st, "qp", [128, 2, SEQ], BF16)
            kp = self.sb(st, "kp", [128, 2, SEQ], BF16)
            qm = [self.sb(st, "qm%d" % i, [128, 2, SEQ], BF16) for i in range(2)]
            for i in range(2):
                S.op('dve', lambda i=i: nc.vector.memset(qm[i][:], 0.0), writes=['qm'])
            vt = [self.sb(st, "vt%d" % i, [128, 4, 72], BF16) for i in range(2)]
            E = [self.sb(st, "E%d" % i, [128, 4, 256], BF16) for i in range(2)]
            ot = [self.sb(st, "ot%d" % i, [128, 260], F32) for i in range(2)]
            for i in range(2):
                S.op('dve', lambda i=i: nc.vector.memset(vt[i][:], 1.0), writes=['vt%d' % i])
            it = 0
            for s_ in range(self.nseq):
                for g, (window, dil) in enumerate(A_GROUPS):
                    if os.environ.get("P3G") and str(g) not in os.environ["P3G"]:
                        continue
                    n = SEQ // dil
                    nb = n // 128
                    t0 = s_ * SEQ
                    S.dma('sp', qn[:], self.fm(dr['AQ'])[:, 2 * g:2 * g + 2, t0:t0 + SEQ], writes=['qn'])
                    S.dma('sp', kn[:], self.fm(dr['AK'])[:, 2 * g:2 * g + 2, t0:t0 + SEQ], writes=['kn'])
                    for c in range(2):
                        S.op('dve', lambda c=c: nc.vector.tensor_copy(qm[0][0:64, c, :].rearrange("p (r j) -> p r j", r=dil), qn[0:64, c, :].rearrange("p (j r) -> p r j", r=dil)), reads=['qn'], writes=['qm'])
                        S.op('pool', lambda c=c: nc.gpsimd.tensor_copy(qm[1][64:128, c, :].rearrange("p (r j) -> p r j", r=dil), qn[64:128, c, :].rearrange("p (j r) -> p r j", r=dil)), reads=['qn'], writes=['qm'])
                    if dil > 1:
                        for c in range(2):
                            S.op('pool', lambda c=c: nc.gpsimd.tensor_copy(kp[:, c, :].rearrange("p (r j) -> p r j", r=dil), kn[:, c, :].rearrange("p (j r) -> p r j", r=dil)), reads=['kn'], writes=['kp'])
                        Kt, qk, kk_ = kp, 'qm', 'kp'
                    else:
                        Kt, qk, kk_ = kn, 'qm', 'kn'
                    for r in range(dil):
                        for qb in range(nb):
                            i2 = it % 2
                            it += 1
                            base = r * n + qb * 128
                            row0 = t0 + r + dil * 128 * qb
                            rows = slice(row0, row0 + dil * 127 + 1, dil)
                            vc, vck = vt[qb % 2], 'vt%d' % (qb % 2)
                            vp, vpk = vt[(qb + 1) % 2], 'vt%d' % ((qb + 1) % 2)
                            SK = os.environ.get('P3SKIP', '')
                            if 'v' not in SK:
                              S.dma('sp', vc[:, :, 0:64], dr['AV'][rows, 256 * g:256 * g + 256].rearrange("p (h d) -> p h d", d=64), writes=[vck])
                            bS = 2 * i2
                            for h4 in range(4):
                                c, po = h4 // 2, (h4 % 2) * 64
                                bk = bS + h4 // 2
                                off = (h4 % 2) * 256
                                if qb > 0 and 's' not in SK:
                                    self.mm_group(self.bank(bk)[:, off:off + 128], [(Kt[:, c, base - 128:base], qm[h4 % 2][:, c, base:base + 128])], [qk, kk_], ('ps', bk))
                                if 's' not in SK:
                                  self.mm_group(self.bank(bk)[:, off + 128:off + 256], [(Kt[:, c, base:base + 128], qm[h4 % 2][:, c, base:base + 128])], [qk, kk_], ('ps', bk))
                            Ek = 'E%d' % i2
                            for hb in range(2):
                                if 'e' not in SK:
                                  S.op('act', lambda i2=i2, bS=bS, hb=hb: nc.scalar.activation(E[i2][:, 2 * hb:2 * hb + 2, :].rearrange("p h q -> p (h q)"), self.bank(bS + hb), AF.Exp, scale=0.125),
                                     reads=[('ps', bS + hb)], writes=[Ek])
                            if 'm' not in SK:
                              S.op('dve', lambda i2=i2: nc.vector.tensor_tensor(E[i2][:], E[i2][:], self.mask4[:], ALU.mult), reads=[Ek, 'const2'], writes=[Ek + 'm'])
                            bO = 4 + i2
                            for h4 in range(4):
                                o_ = self.bank(bO)[:, h4 * 68:h4 * 68 + 65]
                                pairs = []
                                if qb > 0:
                                    pairs.append((E[i2][:, h4, 0:128], vp[:, h4, 0:65]))
                                pairs.append((E[i2][:, h4, 128:256], vc[:, h4, 0:65]))
                                if 'p' not in SK:
                                  self.mm_group(o_, pairs, [Ek + 'm', vck, vpk], ('ps', bO))
                            if 'o' not in SK:
                              S.op('dve', lambda i2=i2, bO=bO: nc.vector.tensor_copy(ot[i2][:].rearrange("p (h c) -> p h c", c=65), self.bank(bO)[:, 0:272].rearrange("p (h c) -> p h c", c=68)[:, :, 0:65]), reads=[('ps', bO)], writes=['ot%d' % i2])
                            if 'o' not in SK:
                              S.dma('pool', dr['AO'][g, rows, :], ot[i2][:], reads=['ot%d' % i2])
        S.barrier()

    def p4(self, l):
        nc, S = self.nc, self.S
        dr = self.dram
        with contextlib.ExitStack() as st:
            a = [self.sb(st, "a%d" % i, [128, 3, 260], F32) for i in range(2)]
            sm = self.sb(st, "sm", [128, 4, 65], F32)
            rd_ = self.sb(st, "rd", [128, 4, 1], F32)
            ya = self.sb(st, "ya", [128, 256], BF16)
            yat = [self.sb(st, "yat%d" % i, [128, 2, TS], BF16) for i in range(2)]
            for tt in range(self.NT):
                for sub in range(4):
                    blk = tt * 4 + sub
                    i2 = blk % 2
                    rows = slice(blk * 128, (blk + 1) * 128)
                    S.dma('sp', a[i2][:], dr['AO'][:, rows, :].rearrange("g p c -> p g c"), writes=['a%d' % i2])
                    smf = sm[:].rearrange("p h c -> p (h c)")
                    S.op('dve', lambda i2=i2: nc.vector.tensor_tensor(smf, a[i2][:, 0, :], a[i2][:, 1, :], ALU.add), reads=['a%d' % i2], writes=['sm0'])
                    S.op('dve', lambda i2=i2: nc.vector.tensor_tensor(smf, smf, a[i2][:, 2, :], ALU.add), reads=['a%d' % i2, 'sm0'], writes=['sm'])
                    S.op('dve', lambda: nc.vector.reciprocal(rd_[:], sm[:, :, 64:65]), reads=['sm'], writes=['rd'])
                    S.op('dve', lambda: nc.vector.tensor_tensor(ya[:].rearrange("p (h d) -> p h d", d=64), sm[:, :, 0:64], rd_[:].to_broadcast([128, 4, 64]), ALU.mult), reads=['sm', 'rd'], writes=['ya'])
                    for c in range(2):
                        self.mm_group(self.bank(c)[:, sub * 128:(sub + 1) * 128], [(ya[:, c * 128:(c + 1) * 128], self.ident[:])], ['ya', 'const'], ('ps', c))
                j2 = tt % 2
                for c in range(2):
                    S.op('act', lambda c=c, j2=j2: nc.scalar.copy(yat[j2][:, c, :], self.bank(c)), reads=[('ps', c)], writes=['yat%d' % j2])
                S.dma('pool', self.fm(dr['YA'])[:, :, tt * TS:(tt + 1) * TS], yat[j2][:], reads=['yat%d' % j2])
        S.barrier()

    def p5(self, l):
        nc, S = self.nc, self.S
        dr = self.dram
        scale = 192.0 ** -0.5
        with contextlib.ExitStack() as st:
            qn = [self.sb(st, "qn%d" % i, [128, SEQ], BF16) for i in range(2)]
            qr = [self.sb(st, "qr%d" % i, [128, SEQ], BF16) for i in range(2)]
            for i in range(2):
                S.op('dve', lambda i=i: nc.vector.memset(qr[i][:], 0.0), writes=['qr%d' % i])
            kn = [self.sb(st, "kn%d" % i, [128, SEQ], BF16) for i in range(2)]
            kr = self.sb(st, "kr", [128, SEQ], BF16)
            vt = [self.sb(st, "vt%d" % i, [128, 32, 136], BF16) for i in range(2)]
            E = [self.sb(st, "E%d" % i, [128, TS], BF16) for i in range(2)]
            rec = self.sb(st, "rec", [128, 4], F32)
            ob = [self.sb(st, "ob%d" % i, [128, 128], BF16) for i in range(2)]
            yc = [self.sb(st, "yc%d" % i, [128, TS], BF16) for i in range(2)]
            for i in range(2):
                S.op('dve', lambda i=i: nc.vector.memset(vt[i][:], 1.0), writes=['vt%d' % i])
            it = 0
            hh = 0
            for s_ in range(self.nseq):
                t0 = s_ * SEQ
                S.dma('sp', kr[:], dr['CKR'][:, t0:t0 + SEQ], writes=['kr'])
                for hd in range(6):
                    b2 = hh % 2
                    hh += 1
                    po = (hd % 2) * 64
                    keys = ['qn%d' % b2, 'qr%d' % b2, 'kn%d' % b2, 'vt%d' % b2, 'kr']
                    S.dma('sp', qn[b2][:], dr['CQN'][hd * 128:(hd + 1) * 128, t0:t0 + SEQ], writes=[keys[0]])
                    S.dma('sp', qr[b2][po:po + 64, :], dr['CQR'][(hd // 2) * 128 + po:(hd // 2) * 128 + po + 64, t0:t0 + SEQ], writes=[keys[1]])
                    S.dma('sp', kn[b2][:], dr['CKN'][hd * 128:(hd + 1) * 128, t0:t0 + SEQ], writes=[keys[2]])
                    S.dma('sp', vt[b2][:, :, 0:128], dr['CV'][t0:t0 + SEQ, hd * 128:(hd + 1) * 128].rearrange("(b p) d -> p b d", p=128), writes=[keys[3]])
                    its = [(qt_, kb_) for qt_ in range(8) for kb_ in range(4 * qt_ + 4)]
                    it0 = it
                    it += len(its)

                    def geom(idx):
                        qt_, kb_ = its[idx]
                        i2_ = (it0 + idx) % 2
                        j0_ = max(0, kb_ - 4 * qt_)
                        return qt_, kb_, i2_, j0_, TS - 128 * j0_, slice(qt_ * TS + 128 * j0_, (qt_ + 1) * TS), slice(kb_ * 128, (kb_ + 1) * 128), 4 + i2_

                    def emit_S(idx):
                        qt_, kb_, i2_, j0_, ncol_, qsl_, ksl_, bS_ = geom(idx)
                        self.mm_group(self.bank(bS_, ncol_), [(kn[b2][:, ksl_], qn[b2][:, qsl_]), (kr[:, ksl_], qr[b2][:, qsl_])], keys, ('ps', bS_))
                    emit_S(0)
                    for idx in range(len(its)):
                        qt, kb, i2, j0, ncol, qsl, ksl, bS = geom(idx)
                        if idx + 1 < len(its):
                            emit_S(idx + 1)
                        Ek = 'E%d' % i2
                        S.op('act', lambda i2=i2, bS=bS, ncol=ncol: nc.scalar.activation(E[i2][:, 0:ncol], self.bank(bS, ncol), AF.Exp, scale=scale), reads=[('ps', bS)], writes=[Ek])
                        if kb >= 4 * qt:
                            S.op('dve', lambda i2=i2: nc.vector.tensor_tensor(E[i2][:, 0:128], E[i2][:, 0:128], self.mcur[:], ALU.mult), reads=[Ek, 'const'], writes=[Ek])
                        for j in range(j0, 4):
                            S.op('pe', lambda j=j, i2=i2, kb=kb, j0=j0, qt=qt: nc.tensor.matmul(self.bank(j, 129), E[i2][:, (j - j0) * 128:(j - j0 + 1) * 128], vt[b2][:, kb, 0:129], start=(kb == 0), stop=(kb == 4 * qt + j)),
                                 reads=[Ek] + keys, writes=[('ps', j)], signal=(kb == 4 * qt + j))
                        if kb != 4 * qt + 3:
                            continue
                        y2 = qt % 2
                        for j in range(4):
                            S.op('dve', lambda j=j: nc.vector.reciprocal(rec[:, j:j + 1], self.bank(j)[:, 128:129]), reads=[('ps', j)], writes=['rec'])
                        for j in range(4):
                            o2 = j % 2
                            S.op('dve', lambda j=j, o2=o2: nc.vector.tensor_scalar(ob[o2][:], self.bank(j, 128), rec[:, j:j + 1], None, ALU.mult), reads=[('ps', j), 'rec'], writes=['ob%d' % o2])
                            self.mm_group(self.bank(6)[:, j * 128:(j + 1) * 128], [(ob[o2][:], self.ident[:])], ['ob%d' % o2, 'const'], ('ps', 6))
                        S.op('act', lambda y2=y2: nc.scalar.copy(yc[y2][:], self.bank(6)), reads=[('ps', 6)], writes=['yc%d' % y2])
                        S.dma('pool', dr['YC'][hd * 128:(hd + 1) * 128, t0 + qt * TS:t0 + (qt + 1) * TS], yc[y2][:], reads=['yc%d' % y2])
        S.barrier()

    def p6(self, l, g_b):
        nc, S = self.nc, self.S
        dr = self.dram
        TL = 256
        NCH = TL // 32

        def oreg(bank0, hd, p):
            b = bank0 + (1 if hd >= 5 else 0)
            o = (hd % 5) * 96 if hd < 5 else (hd - 5) * 96
            return self.ps[0:p, b * 512 + o:b * 512 + o + 96], b
        with contextlib.ExitStack() as st:
            q1 = [self.sb(st, "q1%d" % i, [128, 8, TL], BF16) for i in range(2)]
            k1 = [self.sb(st, "k1%d" % i, [128, 8, TL], BF16) for i in range(2)]
            q2 = [self.sb(st, "q2%d" % i, [128, 8, TL], BF16) for i in range(2)]
            ke = [self.sb(st, "ke%d" % i, [32, NCH, 1024], BF16) for i in range(2)]
            bv = [self.sb(st, "bv%d" % i, [32, NCH, 768], BF16) for i in range(2)]
            bg = [self.sb(st, "bg%d" % i, [32, NCH, 768], BF16) for i in range(2)]
            dec = [self.sb(st, "dec%d" % i, [128, 8, NCH], F32) for i in range(2)]
            gn = self.sb(st, "gn", [32, 768], F32)
            stf = self.sb(st, "stf", [128, 8, 96], F32)
            stb = self.sb(st, "stb", [128, 8, 96], BF16)
            aT = [self.sb(st, "aT%d" % i, [32, 8, 32], BF16) for i in range(2)]
            osb = self.sb(st, "osb", [32, NCH, 768], F32)
            sq = self.sb(st, "sq", [32, NCH, 768], F32)
            ss = self.sb(st, "ss", [32, NCH * 8], F32)
            yb = self.sb(st, "yb", [32, NCH, 768], BF16)
            ybt = [self.sb(st, "ybt%d" % i, [128, 6, TL], BF16) for i in range(2)]
            S.dma('sp', gn[:], g_b, writes=['const'])
            ti = 0
            for s_ in range(self.nseq):
                S.op('dve', lambda: nc.vector.memset(stf[:], 0.0), reads=['stb'], writes=['stf'])
                S.op('pool', lambda: nc.gpsimd.memset(stb[:], 0.0), writes=['stb'])
                for tl in range(SEQ // TL):
                    i2 = ti % 2
                    ti += 1
                    t0 = s_ * SEQ + tl * TL
                    tsl = slice(t0, t0 + TL)
                    kq = ['q1%d' % i2, 'k1%d' % i2, 'q2%d' % i2, 'ke%d' % i2, 'bv%d' % i2, 'bg%d' % i2, 'dec%d' % i2]
                    S.dma('sp', q1[i2][:], self.fm(dr['BQ1'])[:, :, tsl], writes=[kq[0]])
                    S.dma('sp', k1[i2][:], self.fm(dr['BK1'])[:, :, tsl], writes=[kq[1]])
                    S.dma('sp', q2[i2][:], self.fm(dr['BQ2'])[:, :, tsl], writes=[kq[2]])
                    S.dma('sp', ke[i2][:], dr['BKE'][tsl, :].rearrange("(c p) f -> p c f", p=32), writes=[kq[3]])
                    S.dma('sp', bv[i2][:], dr['BV'][tsl, :].rearrange("(c p) f -> p c f", p=32), writes=[kq[4]])
                    S.dma('sp', bg[i2][:], dr['BG'][tsl, :].rearrange("(c p) f -> p c f", p=32), writes=[kq[5]])
                    S.dma('sp', dec[i2][:], dr['BDEC'][:, :, t0 // 32:t0 // 32 + NCH], writes=[kq[6]])
                    def emit_pre(c):
                        cs = slice(c * 32, (c + 1) * 32)
                        a2 = c % 2
                        ub = 3 + 2 * (c % 2)
                        for hd in range(8):
                            self.mm_group(self.ps[0:32, hd * 32:(hd + 1) * 32], [(k1[i2][:, hd, cs], q1[i2][:, hd, cs])], [kq[0], kq[1]], ('ps', 0))
                        S.op('dve', lambda a2=a2: nc.vector.tensor_tensor(aT[a2][:], self.ps[0:32, 0:256].rearrange("p (h l) -> p h l", l=32), self.mask8[:], ALU.mult), reads=[('ps', 0), 'const2'], writes=['aT%d' % a2])
                        for hd in range(8):
                            u_, b = oreg(ub, hd, 128)
                            self.mm_group(u_, [(ke[i2][:, c, hd * 128:(hd + 1) * 128], bv[i2][:, c, hd * 96:(hd + 1) * 96])], [kq[3], kq[4]], ('ps', b))

                    def emit_post(c):
                        cs = slice(c * 32, (c + 1) * 32)
                        a2 = c % 2
                        ub = 3 + 2 * (c % 2)
                        for hd in range(8):
                            o_, b = oreg(1, hd, 32)
                            self.mm_group(o_, [(aT[a2][:, hd, :], bv[i2][:, c, hd * 96:(hd + 1) * 96]), (q2[i2][:, hd, cs], stb[:, hd, :])], ['aT%d' % a2, kq[4], kq[2], 'stb'], ('ps', b))
                        S.op('act', lambda c=c: nc.scalar.copy(osb[:, c, 0:480], self.ps[0:32, 512:512 + 480]), reads=[('ps', 1)], writes=['osb'])
                        S.op('act', lambda c=c: nc.scalar.copy(osb[:, c, 480:768], self.ps[0:32, 1024:1024 + 288]), reads=[('ps', 2)], writes=['osb'])
                        for hd in range(8):
                            u_, b = oreg(ub, hd, 128)
                            S.op('dve', lambda hd=hd, u_=u_, c=c: nc.vector.scalar_tensor_tensor(stf[:, hd, :], stf[:, hd, :], dec[i2][:, hd, c:c + 1], u_, ALU.mult, ALU.add),
                                 reads=[('ps', b), kq[6], 'stf'], writes=['stf'])
                        S.op('act', lambda: nc.scalar.copy(stb[:], stf[:]), reads=['stf'], writes=['stb'])
                    emit_pre(0)
                    for c in range(NCH):
                        if c + 1 < NCH:
                            emit_pre(c + 1)
                        emit_post(c)
                    o4 = osb[:].rearrange("p c (h d) -> p (c h) d", d=96)
                    S.op('pool', lambda: nc.gpsimd.tensor_tensor(sq[:], osb[:], osb[:], ALU.mult), reads=['osb'], writes=['sq'])
                    S.op('dve', lambda: nc.vector.tensor_reduce(ss[:], sq[:].rearrange("p c (h d) -> p (c h) d", d=96), AX.X, ALU.add), reads=['sq'], writes=['ss0'])
                    S.op('dve', lambda: nc.vector.tensor_scalar(ss[:], ss[:], 1.0 / 96, EPS, ALU.mult, ALU.add), reads=['ss0'], writes=['ss1'])
                    S.op('act', lambda: nc.scalar.activation(ss[:], ss[:], AF.Sqrt), reads=['ss1'], writes=['ss2'])
                    S.op('dve', lambda: nc.vector.reciprocal(ss[:], ss[:]), reads=['ss2'], writes=['ss'])
                    S.op('pool', lambda: nc.gpsimd.tensor_tensor(sq[:].rearrange("p c (h d) -> p (c h) d", d=96), o4, ss[:].unsqueeze(2).to_broadcast([32, NCH * 8, 96]), ALU.mult), reads=['osb', 'ss'], writes=['sq'])
                    S.op('pool', lambda: nc.gpsimd.tensor_tensor(sq[:], sq[:], gn[:].unsqueeze(1).to_broadcast([32, NCH, 768]), ALU.mult), reads=['sq', 'const'], writes=['sq'])
                    S.op('pool', lambda: nc.gpsimd.tensor_tensor(yb[:], sq[:], bg[i2][:], ALU.mult), reads=['sq', kq[5]], writes=['yb'])
                    for c in range(NCH):
                        for f in range(6):
                            self.mm_group(self.ps[:, 7 * 512 + f * 32:7 * 512 + (f + 1) * 32], [(yb[:, c, f * 128:(f + 1) * 128], self.ident[0:32, 0:32])], ['yb', 'const'], ('ps', 7))
                        S.op('act', lambda c=c: nc.scalar.copy(ybt[i2][:, :, c * 32:(c + 1) * 32], self.ps[:, 7 * 512:7 * 512 + 192].rearrange("p (f t) -> p f t", t=32)), reads=[('ps', 7)], writes=['ybt%d' % i2])
                    S.dma('pool', self.fm(dr['YB'])[:, :, tsl], ybt[i2][:], reads=['ybt%d' % i2])
        S.barrier()

    def p7(self, l, XT, HT, w_in, w_a, w_b, w_c, w_o):
        nc, S = self.nc, self.S
        dr = self.dram
        with contextlib.ExitStack() as st:
            wg = self.sb(st, "wg", [128, 8, 3072], BF16)
            wa = self.sb(st, "wa", [128, 2, D], BF16)
            wb = self.sb(st, "wb", [128, 6, D], BF16)
            wc = self.sb(st, "wc", [128, 6, D], BF16)
            wo = self.sb(st, "wo", [128, 8, D], BF16)
            ht = [self.sb(st, "ht%d" % i, [128, 8, TS], BF16) for i in range(2)]
            yin = [self.sb(st, "yin%d" % i, [128, 14, TS], BF16) for i in range(2)]
            xt = [self.sb(st, "xt%d" % i, [128, 8, TS], F32) for i in range(2)]
            gt = [self.sb(st, "gt%d" % i, [128, TS], F32) for i in range(2)]
            acc = self.sb(st, "acc", [128, TS], F32)
            tmp = self.sb(st, "tmp", [128, TS], F32)
            mg = self.sb(st, "mg", [128, 8, TS], BF16)
            self.load_w(wg, w_in, O_G, O_G + 3072, 'wg', 8)
            self.load_w(wa, w_a, 0, D, 'wa', 2)
            self.load_w(wb, w_b, 0, D, 'wb', 6)
            self.load_w(wc, w_c, 0, D, 'wc', 6)
            self.load_w(wo, w_o, 0, D, 'wo', 8)
            ysrc = ((dr['YA'], 0, 2, wa), (dr['YB'], 2, 6, wb), (dr['YC'], 8, 6, wc))
            for tt in range(self.NT):
                i2 = tt % 2
                tsl = slice(tt * TS, (tt + 1) * TS)
                h, hk = ht[i2], 'ht%d' % i2
                y, yk = yin[i2], 'yin%d' % i2
                x, xk = xt[i2], 'xt%d' % i2
                S.dma('sp', h[:], self.fm(HT)[:, :, tsl], writes=[hk])
                for (src, o0, n, _) in ysrc:
                    S.dma('sp', y[:, o0:o0 + n, :], self.fm(src)[:, :, tsl], writes=[yk])
                S.dma('sp', x[:], self.fm(XT)[:, :, tsl], writes=[xk])
                for dc in range(8):
                    for bi, (src, o0, n, w_) in enumerate(ysrc):
                        bgt = (dc * 3 + bi) % 2
                        g_ = gt[bgt]
                        gk = 'gt%d' % bgt
                        bG, bP = 2 * bgt, 2 * bgt + 1
                        self.mm_group(self.bank(bG), [(wg[:, kc, bi * D + dc * 128:bi * D + (dc + 1) * 128], h[:, kc, :]) for kc in range(8)], ['wg', hk], ('ps', bG))
                        self.mm_group(self.bank(bP), [(w_[:, kc, dc * 128:(dc + 1) * 128], y[:, o0 + kc, :]) for kc in range(n)], ['wa', 'wb', 'wc', yk], ('ps', bP))
                        S.op('act', lambda g_=g_, bG=bG: nc.scalar.activation(g_[:], self.bank(bG), AF.Sigmoid), reads=[('ps', bG)], writes=[gk])
                        if bi == 0:
                            S.op('dve', lambda g_=g_, bP=bP: nc.vector.tensor_tensor(acc[:], g_[:], self.bank(bP), ALU.mult), reads=[gk, ('ps', bP)], writes=['acc'])
                        else:
                            S.op('dve', lambda g_=g_, bP=bP: nc.vector.tensor_tensor(tmp[:], g_[:], self.bank(bP), ALU.mult), reads=[gk, ('ps', bP)], writes=['tmp'])
                            if bi == 1:
                                S.op('dve', lambda: nc.vector.tensor_tensor(acc[:], acc[:], tmp[:], ALU.add), reads=['acc', 'tmp'], writes=['acc'])
                            else:
                                S.op('dve', lambda dc=dc: nc.vector.tensor_tensor(mg[:, dc, :], acc[:], tmp[:], ALU.add), reads=['acc', 'tmp'], writes=['mg'])
                for dc in range(8):
                    b = 4 + dc % 2
                    self.mm_group(self.bank(b), [(wo[:, kc, dc * 128:(dc + 1) * 128], mg[:, kc, :]) for kc in range(8)], ['wo', 'mg'], ('ps', b))
                    S.op('dve', lambda dc=dc, b=b, x=x: nc.vector.tensor_tensor(x[:, dc, :], x[:, dc, :], self.bank(b), ALU.add), reads=[('ps', b), xk], writes=[xk])
                S.dma('pool', self.fm(XT)[:, :, tsl], x[:], reads=[xk])
        S.barrier()

    def finish(self):
        self.S.barrier()
        self.es.close()


def make_consts():
    import ml_dtypes
    bf = ml_dtypes.bfloat16
    c = {}
    c['ident'] = np.eye(128, dtype=np.float32)
    k = np.arange(128)[:, None]
    q = np.arange(128)[None, :]
    c['mcur'] = (k <= q).astype(np.float32)
    c['mprev'] = (k >= q).astype(np.float32)
    pa = np.zeros((128, 128), np.float32)
    pc = np.zeros((128, 128), np.float32)
    for m in range(128):
        i = m % 64
        if i < 8:
            pa[m + 8, m] = -1.0
        elif i < 16:
            pa[m - 8, m] = 1.0
        if i < 32:
            pc[m + 32, m] = -1.0
        else:
            pc[m - 32, m] = 1.0
    c['permA'] = pa
    c['permC'] = pc
    sm = np.ones((128, TS), np.float32)
    sm[:, ::32] = 0.0
    c['scanm'] = sm
    t = np.arange(SEQ, dtype=np.float32)
    inv_a = (1.0 / (np.float32(500000.0) ** (np.arange(0, 16, 2, dtype=np.float32) / np.float32(16)))).astype(np.float32)
    ang_a = t[:, None] * inv_a[None, :]
    ca = np.ones((128, SEQ), np.float32)
    sa = np.zeros((128, SEQ), np.float32)
    inv_c = (1.0 / (np.float32(10000.0) ** (np.arange(0, 64, 2, dtype=np.float32) / np.float32(64)))).astype(np.float32)
    ang_c = t[:, None] * inv_c[None, :]
    cc = np.zeros((128, SEQ), np.float32)
    sc = np.zeros((128, SEQ), np.float32)
    for m in range(128):
        i = m % 64
        if i < 16:
            ca[m] = np.cos(ang_a[:, i % 8])
            sa[m] = np.sin(ang_a[:, i % 8])
        cc[m] = np.cos(ang_c[:, i % 32])
        sc[m] = np.sin(ang_c[:, i % 32])
    c['ropeA_c'], c['ropeA_s'], c['ropeC_c'], c['ropeC_s'] = ca, sa, cc, sc
    return {k_: np.ascontiguousarray(v.astype(np.float32)) for k_, v in c.items()}


CONST_SHAPES = {'ident': [128, 128], 'mcur': [128, 128], 'mprev': [128, 128], 'permA': [128, 128], 'permC': [128, 128],
                'scanm': [128, TS], 'ropeA_c': [128, SEQ], 'ropeA_s': [128, SEQ], 'ropeC_c': [128, SEQ], 'ropeC_s': [128, SEQ]}

W_SHAPES = {
    'w_ffn1_up': [D, 2 * DFF], 'w_ffn1_down': [DFF, D], 'w_in': [D, 9664], 'w_c_qb': [384, 1152], 'w_c_kvb': [256, 1536],
    'w_branch_a': [256, D], 'w_branch_b': [768, D], 'w_branch_c': [768, D], 'w_out': [D, D],
    'w_ffn2_up': [D, 2 * DFF], 'w_ffn2_down': [DFF, D], 'w_ple_gate': [D, D], 'w_ple_proj': [256, D],
}
V_SHAPES = {
    'norm_ffn1': [128, 8], 'norm_mix': [128, 8], 'norm_ffn2': [128, 8], 'norm_ple': [128, 8],
    'c_q_norm': [128, 3], 'c_kv_norm': [128, 2], 'b_gnorm': [32, 768],
}


def host_layout(inputs, core, nseq, depth):
    m = {}
    xb = inputs['x'][core * nseq:(core + 1) * nseq]
    m['xT'] = np.ascontiguousarray(xb.reshape(nseq * SEQ, D).T)
    pb = inputs['p'][:depth, core * nseq:(core + 1) * nseq]
    m['pT'] = np.ascontiguousarray(pb.reshape(depth, nseq * SEQ, 256).transpose(0, 2, 1))
    for k_ in W_SHAPES:
        m[k_] = np.ascontiguousarray(inputs[k_][:depth])
    for k_ in ('norm_ffn1', 'norm_mix', 'norm_ffn2', 'norm_ple', 'c_q_norm', 'c_kv_norm'):
        v = inputs[k_][:depth]
        m[k_] = np.ascontiguousarray(v.reshape(depth, -1, 128).transpose(0, 2, 1))
    m['norm_final'] = np.ascontiguousarray(inputs['norm_final'].reshape(8, 128).T)
    g = inputs['b_gnorm'][:depth]
    m['b_gnorm'] = np.ascontiguousarray(np.broadcast_to(g[:, None, None, :], (depth, 32, 8, 96)).reshape(depth, 32, 768))
    lbl = inputs['b_lb_logits']
    m['b_lb_logits'] = np.ascontiguousarray(lbl.reshape(lbl.shape[0], 8, 128).transpose(2, 1, 0))
    return m


def build(nseq=2, depth=DEPTH, dump=(), upto=99, consts_only=False):
    nc = bass.Bass("TRN2", target_bir_lowering=False)
    kb = K(nc, nseq, depth, dump)
    T = kb.T
    ein = lambda name, shape, dt=F32: nc.dram_tensor(name, list(shape), dt, kind="ExternalInput").ap()
    cin = {k_: ein(k_, s) for k_, s in CONST_SHAPES.items()}
    xT = ein('xT', [D, T])
    pT = ein('pT', [depth, 256, T])
    W = {k_: ein(k_, [depth] + s) for k_, s in W_SHAPES.items()}
    V = {k_: ein(k_, [depth] + s) for k_, s in V_SHAPES.items()}
    V['norm_final'] = ein('norm_final', [128, 8])
    lbl = ein('b_lb_logits', [128, 8, DEPTH])
    outT = nc.dram_tensor('outT', [D, T], F32, kind="ExternalOutput").ap()
    kb.cin = cin
    kb.lbl = lbl
    kb.setup_consts(cin)
    XT = kb.dt('XT', [D, T], F32)
    HT = kb.dt('HT', [D, T], BF16)
    kb.alloc_scratch()
    kb.S.barrier()
    for l in range(depth):
        last = (l == depth - 1)
        kb.phase_ffn(l, xT if l == 0 else XT, XT, W['w_ffn1_up'][l], W['w_ffn1_down'][l], V['norm_ffn1'][l], V['norm_mix'][l], HT)
        if upto >= 2:
            kb.phase_mixer(l, XT, HT, W, V, lbl)
        if upto >= 3:
            kb.phase_ffn(l, XT, XT, W['w_ffn2_up'][l], W['w_ffn2_down'][l], V['norm_ffn2'][l])
        kb.phase_ple(l, XT, XT, pT[l], W['w_ple_gate'][l], W['w_ple_proj'][l], V['norm_ple'][l],
                     V['norm_final'] if last else None, outT if last else None)
    kb.finish()
    return nc, kb


_CONSTS = None


def kernel(**inputs):
    global _CONSTS
    inputs = {k_: np.asarray(v) for k_, v in inputs.items()}
    if _CONSTS is None:
        _CONSTS = make_consts()
    nseq = 2
    nc, kb = build(nseq=nseq, depth=DEPTH)
    in_maps = []
    for c in range(NCORES):
        m = host_layout(inputs, c, nseq, DEPTH)
        m.update(_CONSTS)
        in_maps.append(m)
    res = run_bass_kernel_spmd(nc, in_maps, core_ids=list(range(NCORES)))
    outs = []
    for c in range(NCORES):
        o = res.results[c]['outT']
        outs.append(np.ascontiguousarray(o.T).reshape(nseq, SEQ, D))
    return np.concatenate(outs, axis=0).astype(np.float32)
```
